# Optimizing a Trainium2 kernel written in Bass

```python
import math
import jax, jax.numpy as jnp
from jax import lax
import numpy as np

D_MODEL = 1024
BATCH = 8
SEQ = 8192
DEPTH = 4

EPS = 1e-6
GLA_HEADS = 4
GLA_DK = 48
GLA_DV = 96
GLA_GATE_RANK = 16
GLA_GATE_NORM = 16.0
GLA_CHUNK = 64
DSA_HEADS = 6
DSA_DH = 64
IDX_HEADS = 4
IDX_DIM = 32
TOPK_MAX = 256
QUERY_BLOCK = 128
POOL_GROUPS = 4
POOL_GC = 64
POOL_WINDOWS = (2, 4, 8, 16)
REL_BUCKETS = 32
REL_MAX_DIST = 128

GLA_W = GLA_HEADS * GLA_DV
DSA_W = DSA_HEADS * DSA_DH
POOL_W = POOL_GROUPS * POOL_GC
D_MIX = GLA_W + DSA_W + POOL_W

IN_SIZES = (
    GLA_HEADS * GLA_DK,
    GLA_HEADS * GLA_DK,
    GLA_W,
    GLA_GATE_RANK,
    GLA_W,
    DSA_W,
    DSA_W,
    DSA_W,
    DSA_W,
    IDX_HEADS * IDX_DIM,
    IDX_DIM,
    IDX_HEADS,
    POOL_W,
    POOL_W,
)
IN_COLS = sum(IN_SIZES)

kernel_name = 'hybrid_gla_dsa_pool'


def split_cols(p):
    outs = []
    off = 0
    for s in IN_SIZES:
        outs.append(p[..., off:off + s])
        off += s
    return outs


def rmsnorm(x, g):
    xf = x.astype(jnp.float32)
    y = xf * lax.rsqrt(jnp.mean(xf * xf, axis=-1, keepdims=True) + EPS)
    return (y * g.astype(jnp.float32)).astype(x.dtype)


def t5_bucket(rel):
    rel = jnp.maximum(rel, 0)
    max_exact = REL_BUCKETS // 2
    relf = jnp.maximum(rel, 1).astype(jnp.float32)
    large = max_exact + (jnp.log(relf / max_exact) / math.log(REL_MAX_DIST / max_exact)
                         * (REL_BUCKETS - max_exact)).astype(jnp.int32)
    large = jnp.minimum(large, REL_BUCKETS - 1)
    return jnp.where(rel < max_exact, rel, large)


def gla_mixer(q, k, v, glog):
    B, T, H, dk = q.shape
    dv = v.shape[-1]
    N = T // GLA_CHUNK
    def chunk(a):
        return a.astype(jnp.float32).reshape(B, N, GLA_CHUNK, H, a.shape[-1])
    qf, kf, vf, gf = chunk(q), chunk(k), chunk(v), chunk(glog)
    b = jnp.cumsum(gf, axis=2)
    b_last = b[:, :, -1:]
    qe = qf * jnp.exp(b) * (dk ** -0.5)
    ke = kf * jnp.exp(-b)
    kd = kf * jnp.exp(b_last - b)
    A = jnp.einsum('bnihd,bnjhd->bnhij', qe, ke)
    tril = jnp.tril(jnp.ones((GLA_CHUNK, GLA_CHUNK), dtype=bool))
    A = jnp.where(tril, A, 0.0)
    o_intra = jnp.einsum('bnhij,bnjhv->bnihv', A, vf)
    U = jnp.einsum('bnjhd,bnjhv->bnhdv', kd, vf)
    decay = jnp.exp(b_last[:, :, 0])
    def step(S, inp):
        dec, u = inp
        return dec[..., None] * S + u, S
    S0 = jnp.zeros((B, H, dk, dv), jnp.float32)
    _, S_prev = lax.scan(step, S0, (jnp.moveaxis(decay, 1, 0), jnp.moveaxis(U, 1, 0)))
    S_prev = jnp.moveaxis(S_prev, 0, 1)
    o_inter = jnp.einsum('bnihd,bnhdv->bnihv', qe, S_prev)
    return (o_intra + o_inter).reshape(B, T, H, dv)


def dsa_mixer(q, k, v, q_idx, k_idx, w_idx, rel_bias):
    B, T, H, dh = q.shape
    topk = min(TOPK_MAX, T // 4)
    NB = T // QUERY_BLOCK
    def blk(a):
        return jnp.moveaxis(a.reshape(B, NB, QUERY_BLOCK, *a.shape[2:]), 1, 0)
    pos_blk = jnp.arange(T, dtype=jnp.int32).reshape(NB, QUERY_BLOCK)
    key_pos = jnp.arange(T, dtype=jnp.int32)
    kidx_f = k_idx.astype(jnp.float32)
    rb = rel_bias.astype(jnp.float32)

    def body(args):
        qb, qib, wb, tq = args
        s = jax.nn.relu(jnp.einsum('bqhd,bsd->bqhs', qib.astype(jnp.float32), kidx_f)
                        * (IDX_DIM ** -0.5))
        score = jnp.einsum('bqhs,bqh->bqs', s, wb.astype(jnp.float32) * (IDX_HEADS ** -0.5))
        visible = key_pos[None, :] <= tq[:, None]
        score = jnp.where(visible[None], score, -jnp.inf)
        _, idx = lax.top_k(score, topk)
        kg = jax.vmap(lambda kk, ii: kk[ii])(k, idx)
        vg = jax.vmap(lambda vv, ii: vv[ii])(v, idx)
        logits = jnp.einsum('bqhd,bqkhd->bhqk', qb.astype(jnp.float32),
                            kg.astype(jnp.float32)) * (dh ** -0.5)
        rel = tq[None, :, None] - idx
        bias = rb[t5_bucket(rel)]
        logits = logits + jnp.moveaxis(bias, -1, 1)
        logits = jnp.where((rel >= 0)[:, None], logits, -1e30)
        p = jax.nn.softmax(logits, axis=-1)
        o = jnp.einsum('bhqk,bqkhd->bqhd', p, vg.astype(jnp.float32))
        return o.astype(q.dtype)

    o = lax.map(body, (blk(q), blk(q_idx), blk(w_idx), pos_blk))
    return jnp.moveaxis(o, 0, 1).reshape(B, T, H * dh)


def pool_mixer(u, pool_w, pool_scale):
    B, T, _ = u.shape
    ug = u.astype(jnp.float32).reshape(B, T, POOL_GROUPS, POOL_GC)
    cs = jnp.cumsum(ug, axis=1)
    pos = jnp.arange(T, dtype=jnp.int32)
    outs = []
    for gi, w in enumerate(POOL_WINDOWS):
        c = cs[:, :, gi]
        lower = jnp.pad(c, ((0, 0), (w, 0), (0, 0)))[:, :T]
        cnt = jnp.minimum(pos + 1, w).astype(jnp.float32)[None, :, None]
        outs.append((c - lower) / cnt - ug[:, :, gi])
    pooled = jnp.stack(outs, axis=2)
    y = jnp.einsum('btgc,gcd->btgd', pooled, pool_w.astype(jnp.float32)).reshape(B, T, POOL_W)
    return (y * pool_scale.astype(jnp.float32)).astype(u.dtype)


def setup_inputs(seed: int = 0) -> dict:
    key = jax.random.key(seed)
    ks = jax.random.split(key, 12)
    f = jnp.float32
    x = jax.random.normal(ks[0], (BATCH, SEQ, D_MODEL), f)
    norm_g = 1.0 + 0.05 * jax.random.normal(ks[1], (DEPTH, D_MODEL), f)
    w_in = jax.random.normal(ks[2], (DEPTH, D_MODEL, IN_COLS), f) * D_MODEL ** -0.5
    gla_gate_w2 = jax.random.normal(ks[3], (DEPTH, GLA_GATE_RANK, GLA_HEADS * GLA_DK), f) * GLA_GATE_RANK ** -0.5
    gla_gate_b = 0.1 * jax.random.normal(ks[4], (DEPTH, GLA_HEADS * GLA_DK), f)
    gla_norm_g = 1.0 + 0.05 * jax.random.normal(ks[5], (DEPTH, GLA_DV), f)
    rel_bias = 0.1 * jax.random.normal(ks[6], (REL_BUCKETS, DSA_HEADS), f)
    pool_w = jax.random.normal(ks[7], (DEPTH, POOL_GROUPS, POOL_GC, POOL_GC), f) * POOL_GC ** -0.5
    pool_scale = 1.0 + 0.1 * jax.random.normal(ks[8], (DEPTH, POOL_W), f)
    w_out = jax.random.normal(ks[9], (DEPTH, D_MIX, D_MODEL), f) * D_MIX ** -0.5
    final_norm_g = 1.0 + 0.05 * jax.random.normal(ks[10], (D_MODEL,), f)
    return {'x': x, 'norm_g': norm_g, 'w_in': w_in, 'gla_gate_w2': gla_gate_w2,
            'gla_gate_b': gla_gate_b, 'gla_norm_g': gla_norm_g, 'rel_bias': rel_bias,
            'pool_w': pool_w, 'pool_scale': pool_scale, 'w_out': w_out,
            'final_norm_g': final_norm_g}


def reference(x, norm_g, w_in, gla_gate_w2, gla_gate_b, gla_norm_g, rel_bias,
              pool_w, pool_scale, w_out, final_norm_g):
    B, T, _ = x.shape
    for l in range(DEPTH):
        h = rmsnorm(x, norm_g[l])
        p = jnp.einsum('btd,dc->btc', h, w_in[l])
        (gq, gk, gv, gz, ggate, dq, dk, dv, dgate,
         qi, ki, wi, pu, pgate) = split_cols(p)
        glog = jax.nn.log_sigmoid((jnp.einsum('btr,rc->btc', gz, gla_gate_w2[l])
                                   + gla_gate_b[l]).astype(jnp.float32)) / GLA_GATE_NORM
        o_gla = gla_mixer(gq.reshape(B, T, GLA_HEADS, GLA_DK),
                          gk.reshape(B, T, GLA_HEADS, GLA_DK),
                          gv.reshape(B, T, GLA_HEADS, GLA_DV),
                          glog.reshape(B, T, GLA_HEADS, GLA_DK))
        o_gla = rmsnorm(o_gla, gla_norm_g[l]).reshape(B, T, GLA_W).astype(x.dtype)
        y_gla = o_gla * jax.nn.silu(ggate)
        o_dsa = dsa_mixer(dq.reshape(B, T, DSA_HEADS, DSA_DH),
                          dk.reshape(B, T, DSA_HEADS, DSA_DH),
                          dv.reshape(B, T, DSA_HEADS, DSA_DH),
                          qi.reshape(B, T, IDX_HEADS, IDX_DIM), ki, wi, rel_bias)
        y_dsa = o_dsa * jax.nn.silu(dgate)
        y_pool = pool_mixer(pu, pool_w[l], pool_scale[l]) * jax.nn.silu(pgate)
        y = jnp.concatenate([y_gla, y_dsa, y_pool], axis=-1)
        x = x + jnp.einsum('btc,cd->btd', y, w_out[l])
    return rmsnorm(x, final_norm_g)
```

```python
import math
from contextlib import ExitStack

import numpy as np
import concourse.bass as bass
import concourse.mybir as mybir
from concourse.bass_utils import run_bass_kernel_spmd

F32 = mybir.dt.float32
BF16 = mybir.dt.bfloat16
AF = mybir.ActivationFunctionType
ALU = mybir.AluOpType
AX = mybir.AxisListType

D = 1024
DEPTH = 4
SEQ = 8192
NCORES = 8
EPS = 1e-6
TOPK = 256
N_BISECT = 16
NEG = -1.0e30

O_GQ, O_GK, O_GV, O_GZ, O_GG = 0, 192, 384, 768, 784
O_DQ, O_DK, O_DV, O_DG = 1168, 1552, 1936, 2320
O_QI, O_KI, O_WI, O_PU, O_PG = 2704, 2832, 2864, 2868, 3124

NFM = 13 * 128
NTM = 2308
TM_GROUPS = [(0, 512), (512, 512), (1024, 388), (1412, 512), (1924, 384)]

C_TRI, C_UT, C_MASK4, C_MD, C_MP, C_MD0, C_ID, C_POW2, C_NEGM = 0, 128, 256, 768, 1280, 1792, 2304, 2432, 2464
NCONST = 2592
P_NG, P_GN, P_PS, P_W2, P_PW = 0, 1024, 1408, 1664, 1920
NPL = 2176
G_BD, G_BP, G_C31, G_FN = 0, 768, 1536, 1544
NGP = 2568


def _pairpad(w48x4):
    r = w48x4.shape[0]
    out = np.zeros((r, 256), np.float32)
    for h in range(4):
        out[:, h * 64:h * 64 + 48] = w48x4[:, h * 48:(h + 1) * 48]
    return out


def _t5_bucket(rel):
    rel = np.maximum(rel, 0)
    relf = np.maximum(rel, 1).astype(np.float32)
    large = 16 + (np.log(relf / np.float32(16)) / np.float32(math.log(128 / 16))
                  * np.float32(16)).astype(np.int32)
    large = np.minimum(large, 31)
    return np.where(rel < 16, rel, large)


def _consts():
    c = np.zeros((128, NCONST), np.float32)
    j = np.arange(128)[:, None]
    i = np.arange(128)[None, :]
    same = (j // 64) == (i // 64)
    c[:, C_TRI:C_TRI + 128] = np.where(same & (j <= i), -1.0 / 16.0, 0.0)
    c[:, C_UT:C_UT + 128] = np.where(same & (j > i), -1.0 / 16.0, 0.0)
    m = np.where(same & (j <= i), 1.0, 0.0)
    for h in range(4):
        c[:, C_MASK4 + h * 128:C_MASK4 + (h + 1) * 128] = m
    s = np.arange(128)[:, None]
    t = np.arange(128)[None, :]
    for g, w in enumerate((2, 4, 8, 16)):
        md = np.where((s <= t) & (s > t - w), 1.0 / w, 0.0) - np.where(s == t, 1.0, 0.0)
        mp = np.where((s - 128) > (t - w), 1.0 / w, 0.0)
        cnt = np.minimum(t + 1, w).astype(np.float32)
        md0 = np.where((s <= t) & (s > t - w), 1.0 / cnt, 0.0) - np.where(s == t, 1.0, 0.0)
        c[:, C_MD + g * 128:C_MD + (g + 1) * 128] = md
        c[:, C_MP + g * 128:C_MP + (g + 1) * 128] = mp
        c[:, C_MD0 + g * 128:C_MD0 + (g + 1) * 128] = md0
    c[:, C_ID:C_ID + 128] = np.eye(128, dtype=np.float32)
    c[:, C_POW2:C_POW2 + 32] = (0.5 ** np.arange(1, 33))[None, :]
    c[:, C_NEGM:C_NEGM + 128] = np.where(i > j, NEG, 0.0)
    return c


def _layer_pack(norm_g, gla_norm_g, pool_scale, w2, gate_b, pool_w):
    p = np.zeros((128, NPL), np.float32)
    p[:, P_NG:P_NG + 1024] = norm_g[None, :]
    p[:, P_GN:P_GN + 384] = np.tile(gla_norm_g, 4)[None, :]
    p[:, P_PS:P_PS + 256] = pool_scale[None, :]
    p[0:16, P_W2:P_W2 + 256] = _pairpad(w2)
    p[32, P_W2:P_W2 + 256] = _pairpad(gate_b[None, :])[0]
    for g in range(4):
        p[0:64, P_PW + g * 64:P_PW + (g + 1) * 64] = pool_w[g]
    return p


def _global_pack(rel_bias, final_norm_g):
    gp = np.zeros((128, NGP), np.float32)
    s = np.arange(128)[:, None]
    t = np.arange(128)[None, :]
    bd = _t5_bucket(t - s)
    bp = _t5_bucket(t - s + 128)
    for h in range(6):
        gp[:, G_BD + h * 128:G_BD + (h + 1) * 128] = rel_bias[bd, h]
        gp[:, G_BP + h * 128:G_BP + (h + 1) * 128] = rel_bias[bp, h]
    gp[:, G_C31:G_C31 + 6] = rel_bias[31][None, :]
    gp[:, G_FN:G_FN + 1024] = final_norm_g[None, :]
    return gp


def _w_layouts(w_in_l):
    wfm = np.zeros((D, NFM), np.float32)
    gq = _pairpad(w_in_l[:, O_GQ:O_GQ + 192])
    gk = _pairpad(w_in_l[:, O_GK:O_GK + 192])
    wfm[:, 0:256] = gq
    wfm[:, 256:512] = gk
    wfm[:, 512:528] = w_in_l[:, O_GZ:O_GZ + 16]
    wfm[:, 640:1024] = w_in_l[:, O_DQ:O_DQ + 384]
    wfm[:, 1024:1408] = w_in_l[:, O_DK:O_DK + 384]
    wfm[:, 1408:1536] = w_in_l[:, O_QI:O_QI + 128]
    for r in range(4):
        wfm[:, 1536 + r * 32:1536 + (r + 1) * 32] = w_in_l[:, O_KI:O_KI + 32]
    wtm = np.zeros((D, NTM), np.float32)
    wtm[:, 0:384] = w_in_l[:, O_GG:O_GG + 384]
    wtm[:, 384:768] = w_in_l[:, O_DG:O_DG + 384]
    wtm[:, 768:1024] = w_in_l[:, O_PG:O_PG + 256]
    wtm[:, 1024:1408] = w_in_l[:, O_GV:O_GV + 384]
    wtm[:, 1408:1412] = w_in_l[:, O_WI:O_WI + 4]
    wtm[:, 1412:1668] = gk
    wtm[:, 1668:1924] = w_in_l[:, O_PU:O_PU + 256]
    wtm[:, 1924:2308] = w_in_l[:, O_DV:O_DV + 384]
    return wfm, wtm


class _Stop(Exception):
    pass


class Res:
    __slots__ = ("name", "w", "r", "excl")

    def __init__(self, name, excl=False):
        self.name = name
        self.w = {}
        self.r = {}
        self.excl = excl


class TK:
    def __init__(self, nc, es):
        self.nc = nc
        self.es = es
        self.eng = {"pe": nc.tensor, "dve": nc.vector, "act": nc.scalar, "pool": nc.gpsimd,
                    "sp": nc.sync}
        self.sem = {}
        self.cnt = {}
        self.seen = {e: {} for e in self.eng}
        for e in ("pe", "dve", "act", "pool"):
            self.sem[e] = es.enter_context(nc.semaphore("sem_" + e))
            self.cnt[e] = 0
        self.ninst = 0

    def _wait(self, e, deps):
        for k, c in deps.items():
            if c > 0 and self.seen[e].get(k, 0) < c:
                self.eng[e].wait_ge(self.sem[k], c)
                self.seen[e][k] = c
                self.ninst += 1

    def _deps(self, e, reads, writes):
        deps = {}
        for r in reads:
            for k, c in r.w.items():
                if k == e and e == "pe":
                    continue
                if deps.get(k, 0) < c:
                    deps[k] = c
        for w in writes:
            for k, c in list(w.w.items()) + list(w.r.items()):
                if k == e and e == "pe":
                    continue
                if deps.get(k, 0) < c:
                    deps[k] = c
        return deps

    def op(self, e, fn, R=(), W=(), serial=False):
        W = list(W) + [r for r in R if r.excl]
        R = [r for r in R if not r.excl]
        deps = self._deps(e, R, W)
        if serial and self.cnt[e] > 0:
            deps[e] = self.cnt[e]
        self._wait(e, deps)
        inst = fn(self.eng[e])
        self.cnt[e] += 1
        c = self.cnt[e]
        inst.then_inc(self.sem[e], 1)
        self.ninst += 1
        for r in R:
            r.r[e] = c
        for w in W:
            w.w = {e: c}
            w.r = {}
        return inst

    def pe(self, fn, R=(), W=(), serial=False):
        return self.op("pe", fn, R, W, serial)

    def dve(self, fn, R=(), W=()):
        return self.op("dve", fn, R, W)

    def act(self, fn, R=(), W=()):
        return self.op("act", fn, R, W)

    def pool(self, fn, R=(), W=()):
        return self.op("pool", fn, R, W)

    def stream(self, name):
        if name not in self.sem:
            self.sem[name] = self.es.enter_context(self.nc.semaphore("ds_" + name))
            self.cnt[name] = 0
        return name

    def dma(self, st, out, in_, R=(), W=(), q="sp"):
        deps = self._deps(q, R, W)
        if self.cnt[st] > 0:
            deps[st] = max(deps.get(st, 0), self.cnt[st])
        self._wait(q, deps)
        inst = self.eng[q].dma_start(out=out, in_=in_)
        self.cnt[st] += 16
        c = self.cnt[st]
        inst.then_inc(self.sem[st], 16)
        self.ninst += 1
        for r in R:
            r.r[st] = c
        for w in W:
            w.w = {st: c}
            w.r = {}

    def barrier(self):
        allc = {k: c for k, c in self.cnt.items() if c > 0}
        for e in self.eng:
            self._wait(e, dict(allc))


import os as _os
_DIS = set(_os.environ.get("DISABLE", "").split(","))
GPE = "dve" if "pool" in _DIS else "pool"


def build(T=SEQ, depth=DEPTH, dbg=False, stop_after=None):
    topk = min(TOPK, T // 4)
    QB0 = topk // 128
    NT = T // 128
    NG = T // 512
    nc = bass.Bass("TRN2", target_bir_lowering=False)
    okind = "ExternalOutput" if dbg else "Internal"

    x_d = nc.dram_tensor("x", [T, D], F32, kind="ExternalInput").ap()
    wfm_d = nc.dram_tensor("wfm", [depth, D, NFM], F32, kind="ExternalInput").ap()
    wtm_d = nc.dram_tensor("wtm", [depth, D, NTM], F32, kind="ExternalInput").ap()
    wout_d = nc.dram_tensor("wout", [depth, D, D], F32, kind="ExternalInput").ap()
    lp_d = nc.dram_tensor("lpack", [depth, 128, NPL], F32, kind="ExternalInput").ap()
    gp_d = nc.dram_tensor("gpack", [128, NGP], F32, kind="ExternalInput").ap()
    cst_d = nc.dram_tensor("cpack", [128, NCONST], F32, kind="ExternalInput").ap()
    y_d = nc.dram_tensor("y", [T, D], F32, kind="ExternalOutput").ap()

    xres_d = nc.dram_tensor("xres", [T, D], F32, kind="Internal").ap()
    sqT_d = nc.dram_tensor("s_qT", [128, 3, T], BF16, kind=okind).ap()
    skT_d = nc.dram_tensor("s_kT", [128, 3, T], BF16, kind=okind).ap()
    sqi_d = nc.dram_tensor("s_qi", [2, 64, T], BF16, kind=okind).ap()
    ski_d = nc.dram_tensor("s_ki", [128, T], BF16, kind=okind).ap()
    sv_d = nc.dram_tensor("s_v", [T, 390], BF16, kind=okind).ap()
    sym_d = nc.dram_tensor("s_ymix", [T, D], BF16, kind=okind).ap()
    swi_d = nc.dram_tensor("s_wi", [T, 4], F32, kind=okind).ap()
    smk_d = nc.dram_tensor("s_mask", [NG, NT, 128, 512], BF16, kind=okind).ap()

    with ExitStack() as es:
        tk = TK(nc, es)

        def sb(name, shape, dt):
            return es.enter_context(nc.sbuf_tensor(name, shape, dt))

        banks = [es.enter_context(nc.psum_tensor(f"bank{i}", [128, 512], F32)) for i in range(8)]
        rbank = [Res(f"bank{i}", excl=True) for i in range(8)]

        r_x = [Res(f"x{b}") for b in range(NT)]
        r_sq = [Res(f"sq{g}") for g in range(NG)]
        r_sk = [Res(f"sk{g}") for g in range(NG)]
        r_sqi = [Res(f"sqi{g}") for g in range(NG)]
        r_ski = [Res(f"ski{g}") for g in range(NG)]
        r_sv = [Res(f"sv{b}") for b in range(NT)]
        r_sym = [Res(f"sym{b}") for b in range(NT)]
        r_swi = [Res(f"swi{b}") for b in range(NT)]
        r_smk = [Res(f"smk{b}") for b in range(NT)]

        cst = sb("cst", [128, NCONST], F32)
        r_cst = Res("cst")
        cbf = sb("cbf", [128, 1664], BF16)
        r_cbf = Res("cbf")
        gpk = sb("gpk", [128, NGP], F32)
        r_gpk = Res("gpk")
        epst = sb("epst", [128, 1], F32)
        r_eps = Res("eps")
        st_c = tk.stream("cst")
        st_g = tk.stream("gpk")
        tk.dma(st_c, cst[:], cst_d[:, :], W=[r_cst])
        tk.dma(st_g, gpk[:], gp_d[:, :], W=[r_gpk])
        tk.dve(lambda e: e.tensor_copy(out=cbf[:], in_=cst[:, C_MD:C_MD + 1664]), R=[r_cst], W=[r_cbf])
        tk.dve(lambda e: e.memset(epst[:], EPS), W=[r_eps])
        ids = sb("ids", [128, 128], BF16)
        r_ids = Res("ids")
        tk.dve(lambda e: e.tensor_scalar(out=ids[:], in0=cst[:, C_ID:C_ID + 128], scalar1=30000.0, scalar2=None,
                                         op0=ALU.mult), R=[r_cst], W=[r_ids])
        MDb = cbf[:, 0:512]
        MPb = cbf[:, 512:1024]
        MD0b = cbf[:, 1024:1536]
        IDb = cbf[:, 1536:1664]
        for h in range(6):
            for off in (G_BD, G_BP):
                sl = gpk[:, off + h * 128:off + (h + 1) * 128]
                tk.dve(lambda e, sl=sl, h=h: e.tensor_scalar(
                    out=sl, in0=sl, scalar1=gpk[:, G_C31 + h:G_C31 + h + 1], scalar2=8.0,
                    op0=ALU.subtract, op1=ALU.mult), R=[r_gpk], W=[r_gpk])

        st_misc = [tk.stream(f"misc{i}") for i in range(4)]

        def layer(l):
            x_src = x_d if l == 0 else xres_d
            last = (l == depth - 1)
            def phase_a(pa):
                def sa(name, shape, dt):
                    return pa.enter_context(nc.sbuf_tensor(f"a{l}_{name}", shape, dt))

                wfm = sa("wfm", [128, 8, NFM], BF16)
                wtm = sa("wtm", [128, 8, NTM], BF16)
                r_w = Res("w")
                lpk = sa("lpk", [128, NPL], F32)
                r_lpk = Res("lpk")
                pwb = sa("pwb", [64, 256], BF16)
                r_pwb = Res("pwb")
                tk.dma(st_misc[0], lpk[:], lp_d[l, :, :], W=[r_lpk])
                tk.act(lambda e: e.copy(out=pwb[:], in_=lpk[0:64, P_PW:P_PW + 256]), R=[r_lpk], W=[r_pwb])
                with ExitStack() as ws:
                    wst = [ws.enter_context(nc.sbuf_tensor(f"a{l}_wst{i}", [128, 8, 256], F32)) for i in range(2)]
                    r_wst = [Res("wst0"), Res("wst1")]
                    st_w = st_misc[1:3]
                    ci = 0
                    for (src, dst, ncol) in ((wfm_d, wfm, NFM), (wtm_d, wtm, NTM)):
                        srcv = src[l].rearrange("(k p) c -> p k c", p=128)
                        c0 = 0
                        while c0 < ncol:
                            cw = min(256, ncol - c0)
                            s = ci % 2
                            tk.dma(st_w[s], wst[s][:, :, 0:cw], srcv[:, :, c0:c0 + cw], W=[r_wst[s]])
                            engs = ("act", "dve", "pool")
                            en = engs[ci % 3]
                            if en == "act":
                                tk.act(lambda e, s=s, cw=cw, c0=c0, dst=dst: e.copy(out=dst[:, :, c0:c0 + cw], in_=wst[s][:, :, 0:cw]),
                                       R=[r_wst[s]], W=[r_w])
                            else:
                                tk.op(en, lambda e, s=s, cw=cw, c0=c0, dst=dst: e.tensor_copy(out=dst[:, :, c0:c0 + cw], in_=wst[s][:, :, 0:cw]),
                                      R=[r_wst[s]], W=[r_w])
                            c0 += cw
                            ci += 1
                    tk.barrier()
                if stop_after == "weights":
                    raise _Stop()
                r_w = Res("w2")

                normg = lpk[:, P_NG:P_NG + 1024]
                gnorm4 = lpk[:, P_GN:P_GN + 384]
                pscale = lpk[:, P_PS:P_PS + 256]
                w2aug = lpk[0:33, P_W2:P_W2 + 256]

                NXS = 2
                xs = [sa(f"x{i}", [128, D], F32) for i in range(NXS)]
                r_xs = [Res(f"xs{i}") for i in range(NXS)]
                st_x = [tk.stream(f"a_x{i}") for i in range(NXS)]
                junk = sa("junk", [128, D], BF16)
                ssq = sa("ssq", [128, 8], F32)
                r_ssq = Res("ssq")
                hb = [sa(f"h{i}", [128, D], BF16) for i in range(2)]
                r_hb = [Res("h0"), Res("h1")]
                hT = [sa(f"hT{i}", [128, 8, 512], BF16) for i in range(2)]
                r_hT = [Res("hT0"), Res("hT1")]
                fmg = [sa(f"fmg{i}", [128, 4, 512], BF16) for i in range(2)]
                r_fmg = [Res("fmg0"), Res("fmg1")]
                gzs = [sa(f"gz{i}", [64, 512], F32) for i in range(2)]
                r_gzs = [Res("gz0"), Res("gz1")]
                fmq = sa("fmq", [128, 3, 512], BF16)
                fmk = sa("fmk", [128, 3, 512], BF16)
                fmqi = sa("fmqi", [128, 512], BF16)
                fmki = sa("fmki", [128, 512], BF16)
                r_fmq, r_fmk, r_fmqi, r_fmki = Res("fmq"), Res("fmk"), Res("fmqi"), Res("fmki")
                st_fm = [tk.stream(f"a_fm{i}") for i in range(4)]
                sg = sa("sg", [128, 1024], F32)
                r_sg = Res("sg")
                gg = sa("gg", [128, 384], F32)
                r_gg = Res("gg")
                psg = sa("psg", [128, 256], F32)
                r_psg = Res("psg")
                vgl = sa("vgl", [128, 384], BF16)
                r_vgl = Res("vgl")
                ktm = sa("ktm", [128, 256], F32)
                r_ktm = Res("ktm")
                pus = [sa(f"pu{i}", [128, 256], BF16) for i in range(2)]
                r_pus = [Res("pu0"), Res("pu1")]
                vaug = [sa(f"vaug{i}", [128, 6, 65], BF16) for i in range(2)]
                r_vaug = [Res("va0"), Res("va1")]
                st_va = [tk.stream(f"a_va{i}") for i in range(2)]
                ymx = [sa(f"ymx{i}", [128, 1024], BF16) for i in range(2)]
                r_ymx = [Res("ym0"), Res("ym1")]
                st_ym = [tk.stream(f"a_ym{i}") for i in range(2)]
                wis = [sa(f"wis{i}", [128, 4], F32) for i in range(2)]
                r_wis = [Res("wi0"), Res("wi1")]
                st_wi = [tk.stream(f"a_wi{i}") for i in range(2)]
                lt = sa("lt", [128, 256], F32)
                r_lt = Res("lt")
                ex = sa("ex", [128, 256], F32)
                r_ex = Res("ex")
                Eq = sa("Eq", [128, 256], F32)
                Ek = sa("Ek", [128, 256], F32)
                Ekd = sa("Ekd", [128, 256], F32)
                r_Eq, r_Ek, r_Ekd = Res("Eq"), Res("Ek"), Res("Ekd")
                qeT = sa("qeT", [128, 2, 128], BF16)
                keT = sa("keT", [128, 2, 128], BF16)
                kd = sa("kd", [128, 256], BF16)
                r_qeT, r_keT, r_kd = Res("qeT"), Res("keT"), Res("kd")
                ATs = sa("ATs", [128, 512], BF16)
                r_ATs = Res("ATs")
                S32 = sa("S32", [128, 2, 96], F32)
                S32m = sa("S32m", [128, 2, 96], F32)
                Sba = sa("Sba", [128, 2, 96], BF16)
                Sbb = sa("Sbb", [128, 2, 96], BF16)
                r_S32, r_S32m, r_Sba, r_Sbb = Res("S32"), Res("S32m"), Res("Sba"), Res("Sbb")
                rt4 = sa("rt4", [128, 8], F32)
                r_rt4 = Res("rt4")
                rstd4 = sa("rstd4", [128, 8], F32)
                r_rstd4 = Res("rstd4")
                pTs = sa("pTs", [64, 512], BF16)
                r_pTs = Res("pTs")

                tk.dve(lambda e: e.memset(S32[:], 0.0), W=[r_S32])
                tk.dve(lambda e: e.memset(Sba[:], 0.0), W=[r_Sba])
                tk.dve(lambda e: e.memset(S32m[:], 0.0), W=[r_S32m])
                tk.dve(lambda e: e.memset(Sbb[:], 0.0), W=[r_Sbb])
                for i in range(2):
                    tk.dve(lambda e, i=i: e.memset(gzs[i][:], 0.0), W=[r_gzs[i]])
                    tk.dve(lambda e, i=i: e.memset(gzs[i][32:33, :], 1.0), W=[r_gzs[i]])
                    tk.dve(lambda e, i=i: e.memset(vaug[i][:], 1.0), W=[r_vaug[i]])

                B_TR, B_FM, B_TM, B_G0, B_G1, B_G2 = 0, (1, 2), (3, 4), 5, 6, 7
                tr_bf = banks[B_TR].bitcast(BF16)

                def load_x(blk):
                    s = blk % NXS
                    tk.dma(st_x[s], xs[s][:], x_src[blk * 128:(blk + 1) * 128, :], R=[r_x[blk]], W=[r_xs[s]])

                for b0 in range(min(NXS, NT)):
                    load_x(b0)

                evac_rr = [0]

                def evac(out, in_, R, W):
                    evac_rr[0] += 1
                    if evac_rr[0] % 2:
                        tk.act(lambda e: e.copy(out=out, in_=in_), R=R, W=W)
                    else:
                        tk.dve(lambda e: e.tensor_copy(out=out, in_=in_), R=R, W=W)

                for tg in range(NG):
                    hs = tg % 2
                    for j in range(4):
                        blk = tg * 4 + j
                        s = blk % NXS
                        hh = blk % 2
                        tk.act(lambda e, s=s: e.activation(out=junk[:], in_=xs[s][:], func=AF.Square,
                                                            accum_out=ssq[:, 0:1]),
                               R=[r_xs[s]], W=[r_ssq])
                        tk.act(lambda e: e.activation(out=ssq[:, 1:2], in_=ssq[:, 0:1], func=AF.Sqrt,
                                                      bias=epst[:, 0:1], scale=1.0 / D),
                               R=[r_ssq, r_eps], W=[r_ssq])
                        tk.dve(lambda e: e.reciprocal(out=ssq[:, 2:3], in_=ssq[:, 1:2]), R=[r_ssq], W=[r_ssq])
                        tk.dve(lambda e, s=s, hh=hh: e.scalar_tensor_tensor(
                            out=hb[hh][:], in0=xs[s][:], scalar=ssq[:, 2:3], in1=normg,
                            op0=ALU.mult, op1=ALU.mult), R=[r_xs[s], r_ssq, r_lpk], W=[r_hb[hh]])
                        if blk + NXS < NT:
                            load_x(blk + NXS)
                        for k in range(8):
                            tk.pe(lambda e, k=k, hh=hh: e.transpose(
                                out=tr_bf[:, k * 128:(k + 1) * 128], in_=hb[hh][:, k * 128:(k + 1) * 128],
                                identity=IDb), R=[r_hb[hh], r_cbf], W=[rbank[B_TR]])
                        evac(hT[hs][:, :, j * 128:(j + 1) * 128],
                             tr_bf[:, :].rearrange("p (k c) -> p k c", k=8),
                             R=[rbank[B_TR]], W=[r_hT[hs]])
                    if stop_after == "norm":
                        raise _Stop()
                    for cb in range(13):
                        bk = B_FM[cb % 2]
                        M = 64 if cb == 4 else 128
                        for k in range(8):
                            tk.pe(lambda e, k=k, cb=cb, bk=bk, M=M: e.matmul(
                                out=banks[bk][0:M, :], lhsT=wfm[:, k, cb * 128:cb * 128 + M],
                                rhs=hT[hs][:, k, :], start=(k == 0), stop=(k == 7)),
                                R=[r_hT[hs], r_w], W=[rbank[bk]])
                        if cb < 4:
                            evac(fmg[hs][:, cb, :], banks[bk][:, :], R=[rbank[bk]], W=[r_fmg[hs]])
                        elif cb == 4:
                            evac(gzs[hs][0:32, :], banks[bk][0:32, :], R=[rbank[bk]], W=[r_gzs[hs]])
                        elif cb < 8:
                            evac(fmq[:, cb - 5, :], banks[bk][:, :], R=[rbank[bk]], W=[r_fmq])
                        elif cb < 11:
                            evac(fmk[:, cb - 8, :], banks[bk][:, :], R=[rbank[bk]], W=[r_fmk])
                        elif cb == 11:
                            evac(fmqi[:], banks[bk][:, :], R=[rbank[bk]], W=[r_fmqi])
                        else:
                            evac(fmki[:], banks[bk][:, :], R=[rbank[bk]], W=[r_fmki])
                    tsl = slice(tg * 512, (tg + 1) * 512)
                    tk.dma(st_fm[0], sqT_d[:, :, tsl], fmq[:], R=[r_fmq], W=[r_sq[tg]])
                    tk.dma(st_fm[1], skT_d[:, :, tsl], fmk[:], R=[r_fmk], W=[r_sk[tg]])
                    tk.dma(st_fm[2], sqi_d[0, :, tsl], fmqi[0:64, :], R=[r_fmqi], W=[r_sqi[tg]])
                    tk.dma(st_fm[2], sqi_d[1, :, tsl], fmqi[64:128, :], R=[r_fmqi], W=[r_sqi[tg]])
                    tk.dma(st_fm[3], ski_d[:, tsl], fmki[:], R=[r_fmki], W=[r_ski[tg]])

                    if stop_after == "fm":
                        raise _Stop()
                    for j in range(4):
                        blk = tg * 4 + j
                        s2 = blk % 2
                        jsl = slice(j * 128, (j + 1) * 128)

                        def tm_mm(gi, bk):
                            c0, cw = TM_GROUPS[gi]
                            for k in range(8):
                                tk.pe(lambda e, k=k: e.matmul(
                                    out=banks[bk][:, 0:cw], lhsT=hT[hs][:, k, jsl],
                                    rhs=wtm[:, k, c0:c0 + cw], start=(k == 0), stop=(k == 7)),
                                    R=[r_hT[hs], r_w], W=[rbank[bk]])

                        for gi in (0, 1):
                            bk = B_TM[gi]
                            tm_mm(gi, bk)
                            tk.act(lambda e, gi=gi, bk=bk: e.activation(
                                out=sg[:, gi * 512:(gi + 1) * 512], in_=banks[bk][:, :], func=AF.Silu),
                                R=[rbank[bk]], W=[r_sg])
                        if stop_after == "tm0":
                            raise _Stop()
                        bk = B_TM[0]
                        tm_mm(2, bk)
                        tk.dve(lambda e, bk=bk: e.tensor_copy(out=vgl[:], in_=banks[bk][:, 0:384]),
                               R=[rbank[bk]], W=[r_vgl])
                        tk.act(lambda e, bk=bk: e.copy(out=wis[s2][:], in_=banks[bk][:, 384:388]),
                               R=[rbank[bk]], W=[r_wis[s2]])
                        if "wi" not in _DIS:
                            tk.dma(st_wi[s2], swi_d[blk * 128:(blk + 1) * 128, :], wis[s2][:], R=[r_wis[s2]], W=[r_swi[blk]])
                        if stop_after == "tm1":
                            raise _Stop()
                        bk = B_TM[1]
                        if "mm3" not in _DIS:
                            tm_mm(3, bk)
                        if "ktm" not in _DIS:
                            tk.act(lambda e, bk=bk: e.copy(out=ktm[:], in_=banks[bk][:, 0:256]),
                                   R=[rbank[bk]], W=[r_ktm])
                        if "pus" not in _DIS:
                            tk.dve(lambda e, bk=bk: e.tensor_copy(out=pus[s2][:], in_=banks[bk][:, 256:512]),
                                   R=[rbank[bk]], W=[r_pus[s2]])
                        if stop_after == "tm2":
                            raise _Stop()
                        bk = B_TM[0]
                        tm_mm(4, bk)
                        tk.act(lambda e, bk=bk: e.copy(
                            out=vaug[s2][:, :, 0:64],
                            in_=banks[bk][:, 0:384].rearrange("p (h d) -> p h d", h=6)),
                            R=[rbank[bk]], W=[r_vaug[s2]])
                        if "va" not in _DIS:
                            tk.dma(st_va[s2], sv_d[blk * 128:(blk + 1) * 128, :],
                                   vaug[s2][:].rearrange("p h d -> p (h d)"), R=[r_vaug[s2]], W=[r_sv[blk]])
                        tk.op(GPE, lambda e: e.tensor_copy(out=ymx[s2][:, 384:768], in_=sg[:, 384:768]),
                                R=[r_sg], W=[r_ymx[s2]])
                        tk.op(GPE, lambda e: e.tensor_tensor(out=gg[:], in0=sg[:, 0:384], in1=gnorm4, op=ALU.mult),
                                R=[r_sg, r_lpk], W=[r_gg])
                        tk.op(GPE, lambda e: e.tensor_tensor(out=psg[:], in0=sg[:, 768:1024], in1=pscale, op=ALU.mult),
                                R=[r_sg, r_lpk], W=[r_psg])

                        if stop_after == "tm":
                            raise _Stop()
                        tk.pe(lambda e: e.matmul(out=banks[B_G0][:, 0:256], lhsT=gzs[hs][0:33, jsl],
                                                 rhs=w2aug, start=True, stop=True),
                              R=[r_gzs[hs], r_lpk], W=[rbank[B_G0]])
                        tk.act(lambda e: e.activation(out=ex[:], in_=banks[B_G0][:, 0:256], func=AF.Exp, scale=-1.0),
                               R=[rbank[B_G0]], W=[r_ex])
                        tk.act(lambda e: e.activation(out=lt[:], in_=ex[:], func=AF.Ln, bias=1.0),
                               R=[r_ex], W=[r_lt])
                        for p in range(2):
                            tk.pe(lambda e, p=p: e.matmul(
                                out=banks[B_G0][:, p * 128:(p + 1) * 128], lhsT=lt[:, p * 128:(p + 1) * 128],
                                rhs=cst[:, C_TRI:C_TRI + 128], start=True, stop=True),
                                R=[r_lt, r_cst], W=[rbank[B_G0]])
                        tk.pe(lambda e: e.matmul(out=banks[B_G0][:, 256:512], lhsT=cst[:, C_UT:C_UT + 128],
                                                 rhs=lt[:], start=True, stop=True),
                              R=[r_lt, r_cst], W=[rbank[B_G0]])
                        tk.act(lambda e: e.activation(out=Eq[:], in_=banks[B_G0][:, 0:256], func=AF.Exp),
                               R=[rbank[B_G0]], W=[r_Eq])
                        tk.act(lambda e: e.activation(out=Ek[:], in_=banks[B_G0][:, 0:256], func=AF.Exp, scale=-1.0),
                               R=[rbank[B_G0]], W=[r_Ek])
                        tk.act(lambda e: e.activation(out=Ekd[:], in_=banks[B_G0][:, 256:512], func=AF.Exp),
                               R=[rbank[B_G0]], W=[r_Ekd])
                        tk.dve(lambda e: e.scalar_tensor_tensor(
                            out=qeT[:], in0=fmg[hs][:, 0:2, jsl], scalar=48.0 ** -0.5,
                            in1=Eq[:].rearrange("p (a t) -> p a t", a=2), op0=ALU.mult, op1=ALU.mult),
                            R=[r_fmg[hs], r_Eq], W=[r_qeT])
                        tk.dve(lambda e: e.tensor_tensor(
                            out=keT[:], in0=fmg[hs][:, 2:4, jsl],
                            in1=Ek[:].rearrange("p (a t) -> p a t", a=2), op=ALU.mult),
                            R=[r_fmg[hs], r_Ek], W=[r_keT])
                        tk.dve(lambda e: e.tensor_tensor(out=kd[:], in0=ktm[:], in1=Ekd[:], op=ALU.mult),
                               R=[r_ktm, r_Ekd], W=[r_kd])

                        def u_mm(c):
                            for h in range(4):
                                p, base = h // 2, 64 * (h % 2)
                                tk.pe(lambda e, h=h, p=p, base=base: e.matmul(
                                    out=banks[B_G0][base:base + 64, c * 192 + p * 96:c * 192 + (p + 1) * 96],
                                    lhsT=kd[c * 64:(c + 1) * 64, h * 64:(h + 1) * 64],
                                    rhs=vgl[c * 64:(c + 1) * 64, h * 96:(h + 1) * 96], start=True, stop=True),
                                    R=[r_kd, r_vgl], W=[rbank[B_G0]])

                        u_mm(0)
                        for p in range(2):
                            tk.dve(lambda e, p=p: e.scalar_tensor_tensor(
                                out=S32m[:, p, :], in0=S32[:, p, :], scalar=Eq[:, p * 128 + 63:p * 128 + 64],
                                in1=banks[B_G0][:, p * 96:(p + 1) * 96], op0=ALU.mult, op1=ALU.add),
                                R=[r_S32, r_Eq, rbank[B_G0]], W=[r_S32m])
                        tk.op(GPE, lambda e: e.tensor_copy(out=Sbb[:], in_=S32m[:]), R=[r_S32m], W=[r_Sbb])
                        for h in range(4):
                            p, base = h // 2, 64 * (h % 2)
                            tk.pe(lambda e, h=h, p=p, base=base: e.matmul(
                                out=banks[B_G1][:, h * 128:(h + 1) * 128], lhsT=keT[base:base + 48, p, :],
                                rhs=qeT[base:base + 48, p, :], start=True, stop=True),
                                R=[r_keT, r_qeT], W=[rbank[B_G1]], serial=(h > 0))
                        tk.dve(lambda e: e.tensor_tensor(out=ATs[:], in0=banks[B_G1][:, :],
                                                         in1=cst[:, C_MASK4:C_MASK4 + 512], op=ALU.mult),
                               R=[rbank[B_G1], r_cst], W=[r_ATs])
                        for h in range(4):
                            p, base = h // 2, 64 * (h % 2)
                            osl = slice(h * 96, (h + 1) * 96)
                            tk.pe(lambda e, h=h, osl=osl: e.matmul(
                                out=banks[B_G2][:, osl], lhsT=ATs[:, h * 128:(h + 1) * 128],
                                rhs=vgl[:, osl], start=True, stop=False),
                                R=[r_ATs, r_vgl], W=[rbank[B_G2]])
                            tk.pe(lambda e, p=p, base=base, osl=osl: e.matmul(
                                out=banks[B_G2][0:64, osl], lhsT=qeT[base:base + 48, p, 0:64],
                                rhs=Sba[base:base + 48, p, :], start=False, stop=True),
                                R=[r_qeT, r_Sba], W=[rbank[B_G2]])
                            tk.pe(lambda e, p=p, base=base, osl=osl: e.matmul(
                                out=banks[B_G2][64:128, osl], lhsT=qeT[base:base + 48, p, 64:128],
                                rhs=Sbb[base:base + 48, p, :], start=False, stop=True),
                                R=[r_qeT, r_Sbb], W=[rbank[B_G2]])
                        u_mm(1)
                        for p in range(2):
                            tk.dve(lambda e, p=p: e.scalar_tensor_tensor(
                                out=S32[:, p, :], in0=S32m[:, p, :], scalar=Eq[:, p * 128 + 127:p * 128 + 128],
                                in1=banks[B_G0][:, 192 + p * 96:192 + (p + 1) * 96], op0=ALU.mult, op1=ALU.add),
                                R=[r_S32m, r_Eq, rbank[B_G0]], W=[r_S32])
                        tk.op(GPE, lambda e: e.tensor_copy(out=Sba[:], in_=S32[:]), R=[r_S32], W=[r_Sba])
                        for h in range(4):
                            tk.act(lambda e, h=h: e.activation(
                                out=junk[:, 0:96], in_=banks[B_G2][:, h * 96:(h + 1) * 96], func=AF.Square,
                                accum_out=rt4[:, h:h + 1]), R=[rbank[B_G2]], W=[r_rt4])
                        tk.act(lambda e: e.activation(out=rt4[:, 4:8], in_=rt4[:, 0:4], func=AF.Sqrt,
                                                      bias=epst[:, 0:1], scale=1.0 / 96.0),
                               R=[r_rt4, r_eps], W=[r_rt4])
                        tk.dve(lambda e: e.reciprocal(out=rstd4[:, 0:4], in_=rt4[:, 4:8]), R=[r_rt4], W=[r_rstd4])
                        for h in range(4):
                            tk.dve(lambda e, h=h: e.scalar_tensor_tensor(
                                out=ymx[s2][:, h * 96:(h + 1) * 96], in0=banks[B_G2][:, h * 96:(h + 1) * 96],
                                scalar=rstd4[:, h:h + 1], in1=gg[:, h * 96:(h + 1) * 96],
                                op0=ALU.mult, op1=ALU.mult),
                                R=[rbank[B_G2], r_rstd4, r_gg], W=[r_ymx[s2]])

                        if stop_after == "gla":
                            raise _Stop()
                        md = MD0b if blk == 0 else MDb
                        for g in range(4):
                            tk.pe(lambda e, g=g: e.matmul(
                                out=banks[B_G1][0:64, g * 128:(g + 1) * 128], lhsT=pus[s2][:, g * 64:(g + 1) * 64],
                                rhs=md[:, g * 128:(g + 1) * 128], start=True, stop=(blk == 0)),
                                R=[r_pus[s2], r_cbf], W=[rbank[B_G1]])
                            if blk > 0:
                                tk.pe(lambda e, g=g: e.matmul(
                                    out=banks[B_G1][0:64, g * 128:(g + 1) * 128],
                                    lhsT=pus[1 - s2][:, g * 64:(g + 1) * 64],
                                    rhs=MPb[:, g * 128:(g + 1) * 128], start=False, stop=True),
                                    R=[r_pus[1 - s2], r_cbf], W=[rbank[B_G1]])
                        tk.act(lambda e: e.copy(out=pTs[:], in_=banks[B_G1][0:64, :]), R=[rbank[B_G1]], W=[r_pTs])
                        for g in range(4):
                            tk.pe(lambda e, g=g: e.matmul(
                                out=banks[B_G1][:, g * 64:(g + 1) * 64], lhsT=pTs[0:64, g * 128:(g + 1) * 128],
                                rhs=pwb[0:64, g * 64:(g + 1) * 64], start=True, stop=True),
                                R=[r_pTs, r_pwb], W=[rbank[B_G1]])
                        tk.dve(lambda e: e.tensor_tensor(out=ymx[s2][:, 768:1024], in0=banks[B_G1][:, 0:256],
                                                         in1=psg[:], op=ALU.mult),
                               R=[rbank[B_G1], r_psg], W=[r_ymx[s2]])
                        tk.dma(st_ym[s2], sym_d[blk * 128:(blk + 1) * 128, :], ymx[s2][:], R=[r_ymx[s2]], W=[r_sym[blk]])
                tk.barrier()

            with ExitStack() as pa:
                try:
                    phase_a(pa)
                except _Stop:
                    stopped[0] = True
            if stopped[0]:
                return
            def phase_b1(pb):
                def sbt(name, shape, dt):
                    return pb.enter_context(nc.sbuf_tensor(f"b{l}_{name}", shape, dt))

                ki4 = sbt("ki4", [128, T], BF16)
                r_ki4 = Res("ki4")
                scores = [sbt(f"scores{i}", [128, T], F32) for i in range(2)]
                r_sc = [Res("scores0"), Res("scores1")]
                junkb = sbt("junkb", [128, T], BF16)
                maskb = sbt("maskb", [128, T], BF16)
                r_mask = Res("maskb")
                mst = [sbt(f"mst{i}", [128, NT, 128], BF16) for i in range(2)]
                r_mst = [Res("mst0"), Res("mst1")]
                st_mst = [tk.stream(f"b_mst{i}") for i in range(2)]
                qis = [sbt(f"qis{i}", [64, 2, 128], BF16) for i in range(2)]
                r_qis = [Res("qis0"), Res("qis1")]
                st_qi = [tk.stream(f"b_qi{i}") for i in range(2)]
                wib = [sbt(f"wib{i}", [128, 4], F32) for i in range(2)]
                r_wib = [Res("wib0"), Res("wib1")]
                st_wib = [tk.stream(f"b_wib{i}") for i in range(2)]
                rh = [sbt(f"rh{i}", [128, 4, 512], BF16) for i in range(2)]
                r_rh = [Res("rh0"), Res("rh1")]
                dgw = [sbt(f"dgw{i}", [128, 4, 128], BF16) for i in range(2)]
                r_dgw = [Res("dgw0"), Res("dgw1")]
                bis = sbt("bis", [128, 8], F32)
                r_bis = Res("bis")
                bw = sbt("bw", [128, 32], F32)
                r_bw = Res("bw")
                st_k = tk.stream(f"b_ki")
                nch = max(1, T // 2048)
                cwk = T // nch
                for c in range(nch):
                    tk.dma(st_k, ki4[:, c * cwk:(c + 1) * cwk], ski_d[:, c * cwk:(c + 1) * cwk],
                           R=r_ski, W=[r_ki4])
                trb = [banks[6].bitcast(BF16), banks[7].bitcast(BF16)]
                rr = [0, 0]
                r_mid, r_cd, r_ca, r_lo, r_g, r_M = (Res("mid"), Res("cd"), Res("ca"), Res("lo"), Res("g"), Res("M"))
                r_thr2 = Res("thr2")
                ACT_SHARE = 0.5

                def gen_scores(qb):
                    s = qb % 2
                    S = (qb + 1) * 128
                    g = qb // 4
                    qsl = slice(qb * 128, (qb + 1) * 128)
                    tk.dma(st_qi[s], qis[s][:], sqi_d[:, :, qsl].rearrange("a p t -> p a t"), R=[r_sqi[g]], W=[r_qis[s]])
                    tk.dma(st_wib[s], wib[s][:], swi_d[qsl, :], R=[r_swi[qb]], W=[r_wib[s]])
                    for h in range(4):
                        tk.op(GPE, lambda e, h=h: e.tensor_scalar(
                            out=dgw[s][:, h, :], in0=cst[:, C_ID:C_ID + 128], scalar1=wib[s][:, h:h + 1],
                            scalar2=None, op0=ALU.mult), R=[r_cst, r_wib[s]], W=[r_dgw[s]])
                    nkt = (S + 511) // 512
                    gt0 = rr[0]
                    rr[0] += nkt

                    def kw_(kt):
                        return min(512, S - kt * 512)

                    def score_mm(kt):
                        for h in range(4):
                            tk.pe(lambda e, h=h: e.matmul(
                                out=banks[h][:, 0:kw_(kt)], lhsT=qis[s][32 * (h % 2):32 * (h % 2) + 32, h // 2, :],
                                rhs=ki4[32 * (h % 2):32 * (h % 2) + 32, kt * 512:kt * 512 + kw_(kt)],
                                start=True, stop=True), R=[r_qis[s], r_ki4], W=[rbank[h]])

                    def relu(kt):
                        r = (gt0 + kt) % 2
                        for h in range(4):
                            tk.act(lambda e, h=h: e.activation(
                                out=rh[r][:, h, 0:kw_(kt)], in_=banks[h][:, 0:kw_(kt)], func=AF.Relu,
                                scale=(32.0 ** -0.5) * 0.5), R=[rbank[h]], W=[r_rh[r]])

                    def diag_mm(kt):
                        r = (gt0 + kt) % 2
                        bk = 4 + r
                        for h in range(4):
                            tk.pe(lambda e, h=h: e.matmul(
                                out=banks[bk][:, 0:kw_(kt)], lhsT=dgw[s][:, h, :], rhs=rh[r][:, h, 0:kw_(kt)],
                                start=(h == 0), stop=(h == 3)), R=[r_dgw[s], r_rh[r]], W=[rbank[bk]])

                    def copy(kt):
                        bk = 4 + (gt0 + kt) % 2
                        tk.act(lambda e: e.copy(out=scores[s][:, kt * 512:kt * 512 + kw_(kt)],
                                                in_=banks[bk][:, 0:kw_(kt)]), R=[rbank[bk]], W=[r_sc[s]])

                    for t in range(nkt + 2):
                        if 0 <= t - 1 < nkt:
                            relu(t - 1)
                        if t < nkt:
                            score_mm(t)
                        if 0 <= t - 1 < nkt:
                            diag_mm(t - 1)
                        if 0 <= t - 2 < nkt:
                            copy(t - 2)
                        yield

                def select(qb, filler):
                    s = qb % 2
                    S = (qb + 1) * 128
                    g, j = qb // 4, qb % 4
                    qsl = slice(qb * 128, (qb + 1) * 128)
                    sc = scores[s]
                    rs = r_sc[s]
                    if qb >= QB0:
                        tk.dve(lambda e: e.tensor_reduce(out=bis[:, 0:1], in_=sc[:, 0:S], axis=AX.X, op=ALU.max,
                                                         apply_absolute_value=True), R=[rs], W=[r_M])
                        tk.dve(lambda e: e.tensor_scalar(out=bw[:, 0:N_BISECT], in0=cst[:, C_POW2:C_POW2 + N_BISECT],
                                                         scalar1=bis[:, 0:1], scalar2=2.0001, op0=ALU.mult, op1=ALU.mult),
                               R=[r_cst, r_M], W=[r_bw])
                        tk.dve(lambda e: e.tensor_scalar(out=bis[:, 1:2], in0=bis[:, 0:1], scalar1=-1.0, scalar2=None,
                                                         op0=ALU.mult), R=[r_M], W=[r_lo])
                    else:
                        tk.dve(lambda e: e.memset(bis[:, 1:2], -1.0e29), W=[r_lo])
                    tk.dve(lambda e: e.tensor_tensor(out=sc[:, qsl], in0=sc[:, qsl],
                                                     in1=cst[:, C_NEGM:C_NEGM + 128], op=ALU.add),
                           R=[rs, r_cst], W=[rs])
                    if qb >= QB0:
                        nkt_next = min(NT, qb + 2) * 128 // 512 + (1 if (min(NT, qb + 2) * 128) % 512 else 0)
                        a_sh = (S * 1.045e-3 + 1.1 - 3.8 * (nkt_next + 2) / N_BISECT - 0.2) / (S * 1.885e-3)
                        a_sh = min(0.7, max(0.1, a_sh + 0.05))
                        S1 = min(S, max(128, int(round(S * (1.0 - a_sh) / 128.0)) * 128))
                        nA = S - S1
                        thr = float(topk) - 0.5 - nA / 2.0
                        tk.dve(lambda e: e.tensor_tensor(out=bis[:, 2:3], in0=bis[:, 1:2], in1=bw[:, 0:1], op=ALU.add),
                               R=[r_lo, r_bw], W=[r_mid])
                        for k in range(N_BISECT):
                            lastk = (k == N_BISECT - 1)
                            if nA > 0:
                                tk.act(lambda e: e.activation(out=junkb[:, S1:S], in_=sc[:, S1:S], func=AF.Sign,
                                                              bias=bis[:, 2:3], scale=-1.0, accum_out=bis[:, 5:6]),
                                       R=[rs, r_mid], W=[r_ca])
                            tk.dve(lambda e: e.tensor_scalar(out=junkb[:, 0:S1], in0=sc[:, 0:S1], scalar1=bis[:, 2:3],
                                                             scalar2=None, op0=ALU.is_ge, op1=ALU.add,
                                                             accum_out=bis[:, 3:4]), R=[rs, r_mid], W=[r_cd])
                            if nA > 0:
                                tk.act(lambda e: e.activation(out=bis[:, 6:7], in_=bis[:, 5:6], func=AF.Identity,
                                                              bias=thr, scale=0.5), R=[r_ca], W=[r_thr2])
                                tk.dve(lambda e, k=k: e.tensor_scalar(out=bis[:, 4:5], in0=bis[:, 3:4], scalar1=bis[:, 6:7],
                                                                      scalar2=bw[:, k:k + 1], op0=ALU.is_ge, op1=ALU.mult),
                                       R=[r_cd, r_bw, r_thr2], W=[r_g])
                            else:
                                tk.dve(lambda e, k=k: e.tensor_scalar(out=bis[:, 4:5], in0=bis[:, 3:4], scalar1=thr,
                                                                      scalar2=bw[:, k:k + 1], op0=ALU.is_ge, op1=ALU.mult),
                                       R=[r_cd, r_bw], W=[r_g])
                            if not lastk:
                                tk.dve(lambda e, k=k: e.scalar_tensor_tensor(
                                    out=bis[:, 2:3], in0=bis[:, 2:3], scalar=bw[:, k + 1:k + 2], in1=bis[:, 4:5],
                                    op0=ALU.subtract, op1=ALU.add), R=[r_mid, r_bw, r_g], W=[r_mid])
                            else:
                                tk.dve(lambda e, k=k: e.scalar_tensor_tensor(
                                    out=bis[:, 1:2], in0=bis[:, 2:3], scalar=bw[:, k:k + 1], in1=bis[:, 4:5],
                                    op0=ALU.subtract, op1=ALU.add), R=[r_mid, r_bw, r_g], W=[r_lo])
                            if filler is not None:
                                next(filler, None)
                    if filler is not None:
                        for _ in filler:
                            pass
                    tk.dve(lambda e: e.tensor_scalar(out=maskb[:, 0:S], in0=sc[:, 0:S], scalar1=bis[:, 1:2],
                                                     scalar2=None, op0=ALU.is_ge), R=[rs, r_lo], W=[r_mask])
                    kb0 = 0
                    while kb0 <= qb:
                        n = min(8, qb + 1 - kb0)
                        t = rr[1] % 2
                        rr[1] += 1
                        for i in range(n):
                            tk.pe(lambda e, i=i: e.transpose(out=trb[t][:, i * 128:(i + 1) * 128],
                                                             in_=maskb[:, (kb0 + i) * 128:(kb0 + i + 1) * 128],
                                                             identity=IDb), R=[r_mask, r_cbf], W=[rbank[6 + t]])
                        tk.act(lambda e: e.copy(out=mst[s][:, kb0:kb0 + n, :],
                                                in_=trb[t][:, 0:n * 128].rearrange("p (k c) -> p k c", k=n)),
                               R=[rbank[6 + t]], W=[r_mst[s]])
                        kb0 += n
                    nkbg = 4 * g + 4
                    if qb + 1 < nkbg:
                        tk.op(GPE, lambda e: e.memset(mst[s][:, qb + 1:nkbg, :], 0.0), W=[r_mst[s]])
                    for k0 in range(0, nkbg, 8):
                        k1 = min(nkbg, k0 + 8)
                        tk.dma(st_mst[s], smk_d[g, k0:k1, :, j * 128:(j + 1) * 128].rearrange("k p q -> p k q"),
                               mst[s][:, k0:k1, :], R=[r_mst[s]], W=[r_smk[qb]])

                for _ in gen_scores(0):
                    pass
                for qb in range(NT):
                    select(qb, gen_scores(qb + 1) if qb + 1 < NT else None)
                tk.barrier()

            def phase_b2(pb):
                def sbt(name, shape, dt):
                    return pb.enter_context(nc.sbuf_tensor(f"c{l}_{name}", shape, dt))

                kT = sbt("kT", [128, 3, T], BF16)
                r_kT = Res("kT")
                vA = sbt("vA", [128, NT, 390], BF16)
                r_vA = Res("vA")
                wout = sbt("wout", [128, 8, D], BF16)
                r_wo = Res("wout")
                st_kv = [tk.stream(f"c_kv{i}") for i in range(2)]
                for p in range(3):
                    tk.dma(st_kv[0], kT[:, p, :], skT_d[:, p, :], R=r_sk, W=[r_kT])
                vsrc = sv_d.rearrange("(b p) c -> p b c", p=128)
                bstep = 8
                for b0 in range(0, NT, bstep):
                    b1 = min(NT, b0 + bstep)
                    tk.dma(st_kv[1], vA[:, b0:b1, :], vsrc[:, b0:b1, :], R=r_sv, W=[r_vA])
                with ExitStack() as ws:
                    wst = [ws.enter_context(nc.sbuf_tensor(f"c{l}_wst{i}", [128, 8, 256], F32)) for i in range(2)]
                    r_wst = [Res("wst0"), Res("wst1")]
                    srcv = wout_d[l].rearrange("(k p) c -> p k c", p=128)
                    for ci in range(4):
                        c0 = ci * 256
                        s = ci % 2
                        tk.dma(st_misc[1 + s], wst[s][:], srcv[:, :, c0:c0 + 256], W=[r_wst[s]])
                        if ci % 2:
                            tk.act(lambda e, s=s, c0=c0: e.copy(out=wout[:, :, c0:c0 + 256], in_=wst[s][:]),
                                   R=[r_wst[s]], W=[r_wo])
                        else:
                            tk.dve(lambda e, s=s, c0=c0: e.tensor_copy(out=wout[:, :, c0:c0 + 256], in_=wst[s][:]),
                                   R=[r_wst[s]], W=[r_wo])
                    tk.barrier()
                r_wo = Res("wout2")
                qTg = [sbt(f"qTg{i}", [128, 3, 512], BF16) for i in range(2)]
                r_qTg = [Res("qTg0"), Res("qTg1")]
                st_q = [tk.stream(f"c_q{i}") for i in range(2)]
                NMP = 2
                mp = [sbt(f"mp{i}", [128, 4, 512], BF16) for i in range(NMP)]
                r_mp = [Res(f"mp{i}") for i in range(NMP)]
                st_mp = [tk.stream(f"c_mp{i}") for i in range(NMP)]
                NE = 4
                esb = [sbt(f"esb{i}", [128, 512], BF16) for i in range(NE)]
                r_esb = [Res(f"esb{i}") for i in range(NE)]
                pmb = [sbt(f"pmb{i}", [128, 512], BF16) for i in range(NE)]
                r_pmb = [Res(f"pmb{i}") for i in range(NE)]
                osb = sbt("osb", [65, 6, 512], F32)
                r_osb = Res("osb")
                rinv = sbt("rinv", [128, 8], F32)
                r_rinv = Res("rinv")
                ymx = [sbt(f"ymx{i}", [128, D], BF16) for i in range(2)]
                r_ymx = [Res("ymx0"), Res("ymx1")]
                st_ym = [tk.stream(f"c_ym{i}") for i in range(2)]
                yT = sbt("yT", [128, 8, 128], BF16)
                r_yT = Res("yT")
                xt = [sbt(f"xt{i}", [128, D], F32) for i in range(2)]
                r_xt = [Res("xt0"), Res("xt1")]
                st_xl = [tk.stream(f"c_xl{i}") for i in range(2)]
                st_xs = [tk.stream(f"c_xs{i}") for i in range(2)]
                fst = sbt("fst", [128, 8], F32)
                r_fst = Res("fst")
                QK = (6, 7)
                qkbf = banks[7].bitcast(BF16)
                cnt = [0, 0]
                pend = []
                for g in range(NG):
                    gs = g % 2
                    nkb = 4 * g + 4
                    tk.dma(st_q[gs], qTg[gs][:], sqT_d[:, :, g * 512:(g + 1) * 512], R=[r_sq[g]], W=[r_qTg[gs]])
                    for kc in range(g + 1):
                        ms = cnt[1] % NMP
                        cnt[1] += 1
                        tk.dma(st_mp[ms], mp[ms][:], smk_d[g, kc * 4:(kc + 1) * 4, :, :].rearrange("k p q -> p k q"),
                               R=r_smk[4 * g:4 * g + 4], W=[r_mp[ms]])
                        for kbi in range(4):
                            kb = kc * 4 + kbi
                            for p in range(3):
                                slots = []
                                for hh in range(2):
                                    h, base = 2 * p + hh, 64 * hh
                                    i = cnt[0]
                                    cnt[0] += 1
                                    qb_ = QK[hh]
                                    es_ = i % NE
                                    slots.append((h, qb_, es_))
                                    tk.pe(lambda e, base=base, qb_=qb_: e.matmul(
                                        out=banks[qb_][:, :], lhsT=kT[base:base + 64, p, kb * 128:(kb + 1) * 128],
                                        rhs=qTg[gs][base:base + 64, p, :], start=True, stop=True),
                                        R=[r_kT, r_qTg[gs]], W=[rbank[qb_]])
                                for (h, qb_, es_) in slots:
                                    for j in range(4):
                                        d = 4 * g + j - kb
                                        if d == 0 or d == 1:
                                            off = G_BD if d == 0 else G_BP
                                            tk.dve(lambda e, j=j, off=off, h=h, qb_=qb_: e.tensor_tensor(
                                                out=banks[qb_][:, j * 128:(j + 1) * 128],
                                                in0=banks[qb_][:, j * 128:(j + 1) * 128],
                                                in1=gpk[:, off + h * 128:off + (h + 1) * 128], op=ALU.add),
                                                R=[r_gpk], W=[rbank[qb_]])
                                    tk.act(lambda e, h=h, qb_=qb_, es_=es_: e.activation(
                                        out=esb[es_][:], in_=banks[qb_][:, :], func=AF.Exp,
                                        bias=gpk[:, G_C31 + h:G_C31 + h + 1], scale=0.125),
                                        R=[rbank[qb_], r_gpk], W=[r_esb[es_]])
                                    tk.dve(lambda e, es_=es_: e.tensor_tensor(out=pmb[es_][:], in0=esb[es_][:],
                                                                              in1=mp[ms][:, kbi, :], op=ALU.mult),
                                           R=[r_esb[es_], r_mp[ms]], W=[r_pmb[es_]])

                                    def pv(h=h, kb=kb, es_=es_):
                                        tk.pe(lambda e: e.matmul(
                                            out=banks[h][0:65, :], lhsT=vA[:, kb, h * 65:(h + 1) * 65], rhs=pmb[es_][:],
                                            start=(kb == 0), stop=(kb == nkb - 1)),
                                            R=[r_vA, r_pmb[es_]], W=[rbank[h]])
                                    pend.append(pv)
                                while len(pend) > 2:
                                    pend.pop(0)()
                    while pend:
                        pend.pop(0)()
                    for h in range(6):
                        if h % 2:
                            tk.act(lambda e, h=h: e.copy(out=osb[0:65, h, :], in_=banks[h][0:65, :]),
                                   R=[rbank[h]], W=[r_osb])
                        else:
                            tk.dve(lambda e, h=h: e.tensor_copy(out=osb[0:65, h, :], in_=banks[h][0:65, :]),
                                   R=[rbank[h]], W=[r_osb])
                    for j in range(4):
                        qb = 4 * g + j
                        s = qb % 2
                        jsl = slice(j * 128, (j + 1) * 128)
                        rsl = slice(qb * 128, (qb + 1) * 128)
                        tk.dma(st_ym[s], ymx[s][:], sym_d[rsl, :], R=[r_sym[qb]], W=[r_ymx[s]])
                        tk.dma(st_xl[s], xt[s][:], x_src[rsl, :], R=[r_x[qb]], W=[r_xt[s]])
                        for h in range(6):
                            tk.pe(lambda e, h=h: e.transpose(out=banks[6][:, h * 65:(h + 1) * 65], in_=osb[0:65, h, jsl],
                                                             identity=cst[0:65, C_ID:C_ID + 65]),
                                  R=[r_osb, r_cst], W=[rbank[6]])
                        tk.dve(lambda e: e.reciprocal(
                            out=rinv[:, 0:6],
                            in_=banks[6][:, 0:390].rearrange("p (h d) -> p h d", d=65)[:, :, 64]),
                            R=[rbank[6]], W=[r_rinv])
                        for h in range(6):
                            ysl = slice(384 + h * 64, 384 + (h + 1) * 64)
                            tk.dve(lambda e, h=h, ysl=ysl: e.scalar_tensor_tensor(
                                out=ymx[s][:, ysl], in0=banks[6][:, h * 65:h * 65 + 64], scalar=rinv[:, h:h + 1],
                                in1=ymx[s][:, ysl], op0=ALU.mult, op1=ALU.mult),
                                R=[rbank[6], r_rinv, r_ymx[s]], W=[r_ymx[s]])
                        for k in range(8):
                            tk.pe(lambda e, k=k: e.transpose(out=qkbf[:, k * 128:(k + 1) * 128],
                                                             in_=ymx[s][:, k * 128:(k + 1) * 128], identity=IDb),
                                  R=[r_ymx[s], r_cbf], W=[rbank[7]])
                        tk.act(lambda e: e.copy(out=yT[:], in_=qkbf[:, :].rearrange("p (k c) -> p k c", k=8)),
                               R=[rbank[7]], W=[r_yT])
                        for n in range(2):
                            for k in range(8):
                                tk.pe(lambda e, n=n, k=k: e.matmul(
                                    out=banks[n][:, :], lhsT=yT[:, k, :], rhs=wout[:, k, n * 512:(n + 1) * 512],
                                    start=(k == 0), stop=(k == 7)), R=[r_yT, r_wo], W=[rbank[n]])
                            tk.dve(lambda e, n=n: e.tensor_tensor(
                                out=xt[s][:, n * 512:(n + 1) * 512], in0=xt[s][:, n * 512:(n + 1) * 512],
                                in1=banks[n][:, :], op=ALU.add), R=[rbank[n], r_xt[s]], W=[r_xt[s]])
                        if not last:
                            tk.dma(st_xs[s], xres_d[rsl, :], xt[s][:], R=[r_xt[s]], W=[r_x[qb]])
                        else:
                            tk.act(lambda e: e.activation(out=yT[:].rearrange("p k c -> p (k c)"), in_=xt[s][:],
                                                          func=AF.Square, accum_out=fst[:, 0:1]),
                                   R=[r_xt[s]], W=[r_fst, r_yT])
                            tk.act(lambda e: e.activation(out=fst[:, 1:2], in_=fst[:, 0:1], func=AF.Sqrt,
                                                          bias=epst[:, 0:1], scale=1.0 / D),
                                   R=[r_fst, r_eps], W=[r_fst])
                            tk.dve(lambda e: e.reciprocal(out=fst[:, 2:3], in_=fst[:, 1:2]), R=[r_fst], W=[r_fst])
                            tk.dve(lambda e: e.scalar_tensor_tensor(
                                out=xt[s][:], in0=xt[s][:], scalar=fst[:, 2:3], in1=gpk[:, G_FN:G_FN + D],
                                op0=ALU.mult, op1=ALU.mult), R=[r_xt[s], r_fst, r_gpk], W=[r_xt[s]])
                            tk.dma(st_xs[s], y_d[rsl, :], xt[s][:], R=[r_xt[s]], W=[r_x[qb]])
                tk.barrier()

            if stop_after == "A":
                stopped[0] = True
                return
            with ExitStack() as pb:
                phase_b1(pb)
            if stop_after == "B1":
                stopped[0] = True
                return
            with ExitStack() as pb:
                phase_b2(pb)

        stopped = [False]
        for l in range(depth if stop_after != "init" else 0):
            layer(l)
            if stopped[0]:
                break

        tk.barrier()
    print("instructions emitted:", tk.ninst)
    return nc


def prep_inputs(x, norm_g, w_in, gla_gate_w2, gla_gate_b, gla_norm_g, rel_bias, pool_w, pool_scale,
                w_out, final_norm_g, depth=DEPTH):
    f = np.float32
    w_in = np.asarray(w_in, f)
    wfm = np.zeros((depth, D, NFM), f)
    wtm = np.zeros((depth, D, NTM), f)
    lp = np.zeros((depth, 128, NPL), f)
    for l in range(depth):
        wfm[l], wtm[l] = _w_layouts(w_in[l])
        lp[l] = _layer_pack(np.asarray(norm_g[l], f), np.asarray(gla_norm_g[l], f),
                            np.asarray(pool_scale[l], f), np.asarray(gla_gate_w2[l], f),
                            np.asarray(gla_gate_b[l], f), np.asarray(pool_w[l], f))
    gp = _global_pack(np.asarray(rel_bias, f), np.asarray(final_norm_g, f))
    shared = {"wfm": wfm, "wtm": wtm, "wout": np.ascontiguousarray(np.asarray(w_out, f)[:depth]),
              "lpack": lp, "gpack": gp, "cpack": _consts()}
    return shared


def kernel(x, norm_g, w_in, gla_gate_w2, gla_gate_b, gla_norm_g, rel_bias, pool_w, pool_scale,
           w_out, final_norm_g):
    x = np.asarray(x, np.float32)
    B, T, _ = x.shape
    shared = prep_inputs(x, norm_g, w_in, gla_gate_w2, gla_gate_b, gla_norm_g, rel_bias, pool_w,
                         pool_scale, w_out, final_norm_g)
    nc = build(T=T, depth=DEPTH)
    in_maps = [dict(shared, x=np.ascontiguousarray(x[b])) for b in range(B)]
    res = run_bass_kernel_spmd(nc, in_maps, core_ids=list(range(B)))
    return np.stack([np.asarray(r["y"], np.float32) for r in res.results], axis=0)
```

```python
import math
from contextlib import ExitStack

import numpy as np
import concourse.bass as bass
import concourse.mybir as mybir
from concourse.bass_utils import run_bass_kernel_spmd

F32 = mybir.dt.float32
BF16 = mybir.dt.bfloat16
AF = mybir.ActivationFunctionType
ALU = mybir.AluOpType
AX = mybir.AxisListType

D = 1024
DEPTH = 4
SEQ = 8192
NCORES = 8
EPS = 1e-6
TOPK = 256
N_BISECT = 16
NEG = -1.0e30

O_GQ, O_GK, O_GV, O_GZ, O_GG = 0, 192, 384, 768, 784
O_DQ, O_DK, O_DV, O_DG = 1168, 1552, 1936, 2320
O_QI, O_KI, O_WI, O_PU, O_PG = 2704, 2832, 2864, 2868, 3124

NFM = 13 * 128
NTM = 2308
TM_GROUPS = [(0, 512), (512, 512), (1024, 388), (1412, 512), (1924, 384)]

C_TRI, C_UT, C_MASK4, C_MD, C_MP, C_MD0, C_ID, C_POW2, C_NEGM = 0, 128, 256, 768, 1280, 1792, 2304, 2432, 2464
NCONST = 2592
P_NG, P_GN, P_PS, P_W2, P_PW = 0, 1024, 1408, 1664, 1920
NPL = 2176
G_BD, G_BP, G_C31, G_FN = 0, 768, 1536, 1544
NGP = 2568


def _pairpad(w48x4):
    r = w48x4.shape[0]
    out = np.zeros((r, 256), np.float32)
    for h in range(4):
        out[:, h * 64:h * 64 + 48] = w48x4[:, h * 48:(h + 1) * 48]
    return out


def _t5_bucket(rel):
    rel = np.maximum(rel, 0)
    relf = np.maximum(rel, 1).astype(np.float32)
    large = 16 + (np.log(relf / np.float32(16)) / np.float32(math.log(128 / 16))
                  * np.float32(16)).astype(np.int32)
    large = np.minimum(large, 31)
    return np.where(rel < 16, rel, large)


def _consts():
    c = np.zeros((128, NCONST), np.float32)
    j = np.arange(128)[:, None]
    i = np.arange(128)[None, :]
    same = (j // 64) == (i // 64)
    c[:, C_TRI:C_TRI + 128] = np.where(same & (j <= i), -1.0 / 16.0, 0.0)
    c[:, C_UT:C_UT + 128] = np.where(same & (j > i), -1.0 / 16.0, 0.0)
    m = np.where(same & (j <= i), 1.0, 0.0)
    for h in range(4):
        c[:, C_MASK4 + h * 128:C_MASK4 + (h + 1) * 128] = m
    s = np.arange(128)[:, None]
    t = np.arange(128)[None, :]
    for g, w in enumerate((2, 4, 8, 16)):
        md = np.where((s <= t) & (s > t - w), 1.0 / w, 0.0) - np.where(s == t, 1.0, 0.0)
        mp = np.where((s - 128) > (t - w), 1.0 / w, 0.0)
        cnt = np.minimum(t + 1, w).astype(np.float32)
        md0 = np.where((s <= t) & (s > t - w), 1.0 / cnt, 0.0) - np.where(s == t, 1.0, 0.0)
        c[:, C_MD + g * 128:C_MD + (g + 1) * 128] = md
        c[:, C_MP + g * 128:C_MP + (g + 1) * 128] = mp
        c[:, C_MD0 + g * 128:C_MD0 + (g + 1) * 128] = md0
    c[:, C_ID:C_ID + 128] = np.eye(128, dtype=np.float32)
    c[:, C_POW2:C_POW2 + 32] = (0.5 ** np.arange(1, 33))[None, :]
    c[:, C_NEGM:C_NEGM + 128] = np.where(i > j, NEG, 0.0)
    return c


def _layer_pack(norm_g, gla_norm_g, pool_scale, w2, gate_b, pool_w):
    p = np.zeros((128, NPL), np.float32)
    p[:, P_NG:P_NG + 1024] = norm_g[None, :]
    p[:, P_GN:P_GN + 384] = np.tile(gla_norm_g, 4)[None, :]
    p[:, P_PS:P_PS + 256] = pool_scale[None, :]
    p[0:16, P_W2:P_W2 + 256] = _pairpad(w2)
    p[32, P_W2:P_W2 + 256] = _pairpad(gate_b[None, :])[0]
    for g in range(4):
        p[0:64, P_PW + g * 64:P_PW + (g + 1) * 64] = pool_w[g]
    return p


def _global_pack(rel_bias, final_norm_g):
    gp = np.zeros((128, NGP), np.float32)
    s = np.arange(128)[:, None]
    t = np.arange(128)[None, :]
    bd = _t5_bucket(t - s)
    bp = _t5_bucket(t - s + 128)
    for h in range(6):
        gp[:, G_BD + h * 128:G_BD + (h + 1) * 128] = rel_bias[bd, h]
        gp[:, G_BP + h * 128:G_BP + (h + 1) * 128] = rel_bias[bp, h]
    gp[:, G_C31:G_C31 + 6] = rel_bias[31][None, :]
    gp[:, G_FN:G_FN + 1024] = final_norm_g[None, :]
    return gp


def _w_layouts(w_in_l):
    wfm = np.zeros((D, NFM), np.float32)
    gq = _pairpad(w_in_l[:, O_GQ:O_GQ + 192])
    gk = _pairpad(w_in_l[:, O_GK:O_GK + 192])
    wfm[:, 0:256] = gq
    wfm[:, 256:512] = gk
    wfm[:, 512:528] = w_in_l[:, O_GZ:O_GZ + 16]
    wfm[:, 640:1024] = w_in_l[:, O_DQ:O_DQ + 384]
    wfm[:, 1024:1408] = w_in_l[:, O_DK:O_DK + 384]
    wfm[:, 1408:1536] = w_in_l[:, O_QI:O_QI + 128]
    for r in range(4):
        wfm[:, 1536 + r * 32:1536 + (r + 1) * 32] = w_in_l[:, O_KI:O_KI + 32]
    wtm = np.zeros((D, NTM), np.float32)
    wtm[:, 0:384] = w_in_l[:, O_GG:O_GG + 384]
    wtm[:, 384:768] = w_in_l[:, O_DG:O_DG + 384]
    wtm[:, 768:1024] = w_in_l[:, O_PG:O_PG + 256]
    wtm[:, 1024:1408] = w_in_l[:, O_GV:O_GV + 384]
    wtm[:, 1408:1412] = w_in_l[:, O_WI:O_WI + 4]
    wtm[:, 1412:1668] = gk
    wtm[:, 1668:1924] = w_in_l[:, O_PU:O_PU + 256]
    wtm[:, 1924:2308] = w_in_l[:, O_DV:O_DV + 384]
    return wfm, wtm


class _Stop(Exception):
    pass


class Res:
    __slots__ = ("name", "w", "r", "excl")

    def __init__(self, name, excl=False):
        self.name = name
        self.w = {}
        self.r = {}
        self.excl = excl


class TK:
    def __init__(self, nc, es):
        self.nc = nc
        self.es = es
        self.eng = {"pe": nc.tensor, "dve": nc.vector, "act": nc.scalar, "pool": nc.gpsimd,
                    "sp": nc.sync}
        self.sem = {}
        self.cnt = {}
        self.seen = {e: {} for e in self.eng}
        for e in ("pe", "dve", "act", "pool"):
            self.sem[e] = es.enter_context(nc.semaphore("sem_" + e))
            self.cnt[e] = 0
        self.ninst = 0

    def _wait(self, e, deps):
        for k, c in deps.items():
            if c > 0 and self.seen[e].get(k, 0) < c:
                self.eng[e].wait_ge(self.sem[k], c)
                self.seen[e][k] = c
                self.ninst += 1

    def _deps(self, e, reads, writes):
        deps = {}
        for r in reads:
            for k, c in r.w.items():
                if k == e and e == "pe":
                    continue
                if deps.get(k, 0) < c:
                    deps[k] = c
        for w in writes:
            for k, c in list(w.w.items()) + list(w.r.items()):
                if k == e and e == "pe":
                    continue
                if deps.get(k, 0) < c:
                    deps[k] = c
        return deps

    def op(self, e, fn, R=(), W=(), serial=False):
        W = list(W) + [r for r in R if r.excl]
        R = [r for r in R if not r.excl]
        deps = self._deps(e, R, W)
        if serial and self.cnt[e] > 0:
            deps[e] = self.cnt[e]
        self._wait(e, deps)
        inst = fn(self.eng[e])
        self.cnt[e] += 1
        c = self.cnt[e]
        inst.then_inc(self.sem[e], 1)
        self.ninst += 1
        for r in R:
            r.r[e] = c
        for w in W:
            w.w = {e: c}
            w.r = {}
        return inst

    def pe(self, fn, R=(), W=(), serial=False):
        return self.op("pe", fn, R, W, serial)

    def dve(self, fn, R=(), W=()):
        return self.op("dve", fn, R, W)

    def act(self, fn, R=(), W=()):
        return self.op("act", fn, R, W)

    def pool(self, fn, R=(), W=()):
        return self.op("pool", fn, R, W)

    def stream(self, name):
        if name not in self.sem:
            self.sem[name] = self.es.enter_context(self.nc.semaphore("ds_" + name))
            self.cnt[name] = 0
        return name

    def dma(self, st, out, in_, R=(), W=(), q="sp"):
        deps = self._deps(q, R, W)
        if self.cnt[st] > 0:
            deps[st] = max(deps.get(st, 0), self.cnt[st])
        self._wait(q, deps)
        inst = self.eng[q].dma_start(out=out, in_=in_)
        self.cnt[st] += 16
        c = self.cnt[st]
        inst.then_inc(self.sem[st], 16)
        self.ninst += 1
        for r in R:
            r.r[st] = c
        for w in W:
            w.w = {st: c}
            w.r = {}

    def barrier(self):
        allc = {k: c for k, c in self.cnt.items() if c > 0}
        for e in self.eng:
            self._wait(e, dict(allc))


import os as _os
_DIS = set(_os.environ.get("DISABLE", "").split(","))
GPE = "dve" if "pool" in _DIS else "pool"


def build(T=SEQ, depth=DEPTH, dbg=False, stop_after=None):
    topk = min(TOPK, T // 4)
    QB0 = topk // 128
    NT = T // 128
    NG = T // 512
    nc = bass.Bass("TRN2", target_bir_lowering=False)
    okind = "ExternalOutput" if dbg else "Internal"

    x_d = nc.dram_tensor("x", [T, D], F32, kind="ExternalInput").ap()
    wfm_d = nc.dram_tensor("wfm", [depth, D, NFM], F32, kind="ExternalInput").ap()
    wtm_d = nc.dram_tensor("wtm", [depth, D, NTM], F32, kind="ExternalInput").ap()
    wout_d = nc.dram_tensor("wout", [depth, D, D], F32, kind="ExternalInput").ap()
    lp_d = nc.dram_tensor("lpack", [depth, 128, NPL], F32, kind="ExternalInput").ap()
    gp_d = nc.dram_tensor("gpack", [128, NGP], F32, kind="ExternalInput").ap()
    cst_d = nc.dram_tensor("cpack", [128, NCONST], F32, kind="ExternalInput").ap()
    y_d = nc.dram_tensor("y", [T, D], F32, kind="ExternalOutput").ap()

    xres_d = nc.dram_tensor("xres", [T, D], F32, kind="Internal").ap()
    sqT_d = nc.dram_tensor("s_qT", [128, 3, T], BF16, kind=okind).ap()
    skT_d = nc.dram_tensor("s_kT", [128, 3, T], BF16, kind=okind).ap()
    sqi_d = nc.dram_tensor("s_qi", [2, 64, T], BF16, kind=okind).ap()
    ski_d = nc.dram_tensor("s_ki", [128, T], BF16, kind=okind).ap()
    sv_d = nc.dram_tensor("s_v", [T, 390], BF16, kind=okind).ap()
    sym_d = nc.dram_tensor("s_ymix", [T, D], BF16, kind=okind).ap()
    swi_d = nc.dram_tensor("s_wi", [T, 4], F32, kind=okind).ap()
    smk_d = nc.dram_tensor("s_mask", [NG, NT, 128, 512], BF16, kind=okind).ap()

    with ExitStack() as es:
        tk = TK(nc, es)

        def sb(name, shape, dt):
            return es.enter_context(nc.sbuf_tensor(name, shape, dt))

        banks = [es.enter_context(nc.psum_tensor(f"bank{i}", [128, 512], F32)) for i in range(8)]
        rbank = [Res(f"bank{i}", excl=True) for i in range(8)]

        r_x = [Res(f"x{b}") for b in range(NT)]
        r_sq = [Res(f"sq{g}") for g in range(NG)]
        r_sk = [Res(f"sk{g}") for g in range(NG)]
        r_sqi = [Res(f"sqi{g}") for g in range(NG)]
        r_ski = [Res(f"ski{g}") for g in range(NG)]
        r_sv = [Res(f"sv{b}") for b in range(NT)]
        r_sym = [Res(f"sym{b}") for b in range(NT)]
        r_swi = [Res(f"swi{b}") for b in range(NT)]
        r_smk = [Res(f"smk{b}") for b in range(NT)]

        cst = sb("cst", [128, NCONST], F32)
        r_cst = Res("cst")
        cbf = sb("cbf", [128, 1664], BF16)
        r_cbf = Res("cbf")
        gpk = sb("gpk", [128, NGP], F32)
        r_gpk = Res("gpk")
        epst = sb("epst", [128, 1], F32)
        r_eps = Res("eps")
        st_c = tk.stream("cst")
        st_g = tk.stream("gpk")
        tk.dma(st_c, cst[:], cst_d[:, :], W=[r_cst])
        tk.dma(st_g, gpk[:], gp_d[:, :], W=[r_gpk])
        tk.dve(lambda e: e.tensor_copy(out=cbf[:], in_=cst[:, C_MD:C_MD + 1664]), R=[r_cst], W=[r_cbf])
        tk.dve(lambda e: e.memset(epst[:], EPS), W=[r_eps])
        ids = sb("ids", [128, 128], BF16)
        r_ids = Res("ids")
        tk.dve(lambda e: e.tensor_scalar(out=ids[:], in0=cst[:, C_ID:C_ID + 128], scalar1=30000.0, scalar2=None,
                                         op0=ALU.mult), R=[r_cst], W=[r_ids])
        MDb = cbf[:, 0:512]
        MPb = cbf[:, 512:1024]
        MD0b = cbf[:, 1024:1536]
        IDb = cbf[:, 1536:1664]
        for h in range(6):
            for off in (G_BD, G_BP):
                sl = gpk[:, off + h * 128:off + (h + 1) * 128]
                tk.dve(lambda e, sl=sl, h=h: e.tensor_scalar(
                    out=sl, in0=sl, scalar1=gpk[:, G_C31 + h:G_C31 + h + 1], scalar2=8.0,
                    op0=ALU.subtract, op1=ALU.mult), R=[r_gpk], W=[r_gpk])

        st_misc = [tk.stream(f"misc{i}") for i in range(4)]

        def layer(l):
            x_src = x_d if l == 0 else xres_d
            last = (l == depth - 1)
            def phase_a(pa):
                def sa(name, shape, dt):
                    return pa.enter_context(nc.sbuf_tensor(f"a{l}_{name}", shape, dt))

                wfm = sa("wfm", [128, 8, NFM], BF16)
                wtm = sa("wtm", [128, 8, NTM], BF16)
                r_w = Res("w")
                lpk = sa("lpk", [128, NPL], F32)
                r_lpk = Res("lpk")
                pwb = sa("pwb", [64, 256], BF16)
                r_pwb = Res("pwb")
                tk.dma(st_misc[0], lpk[:], lp_d[l, :, :], W=[r_lpk])
                tk.act(lambda e: e.copy(out=pwb[:], in_=lpk[0:64, P_PW:P_PW + 256]), R=[r_lpk], W=[r_pwb])
                with ExitStack() as ws:
                    wst = [ws.enter_context(nc.sbuf_tensor(f"a{l}_wst{i}", [128, 8, 256], F32)) for i in range(2)]
                    r_wst = [Res("wst0"), Res("wst1")]
                    st_w = st_misc[1:3]
                    ci = 0
                    for (src, dst, ncol) in ((wfm_d, wfm, NFM), (wtm_d, wtm, NTM)):
                        srcv = src[l].rearrange("(k p) c -> p k c", p=128)
                        c0 = 0
                        while c0 < ncol:
                            cw = min(256, ncol - c0)
                            s = ci % 2
                            tk.dma(st_w[s], wst[s][:, :, 0:cw], srcv[:, :, c0:c0 + cw], W=[r_wst[s]])
                            engs = ("act", "dve", "pool")
                            en = engs[ci % 3]
                            if en == "act":
                                tk.act(lambda e, s=s, cw=cw, c0=c0, dst=dst: e.copy(out=dst[:, :, c0:c0 + cw], in_=wst[s][:, :, 0:cw]),
                                       R=[r_wst[s]], W=[r_w])
                            else:
                                tk.op(en, lambda e, s=s, cw=cw, c0=c0, dst=dst: e.tensor_copy(out=dst[:, :, c0:c0 + cw], in_=wst[s][:, :, 0:cw]),
                                      R=[r_wst[s]], W=[r_w])
                            c0 += cw
                            ci += 1
                    tk.barrier()
                if stop_after == "weights":
                    raise _Stop()
                r_w = Res("w2")

                normg = lpk[:, P_NG:P_NG + 1024]
                gnorm4 = lpk[:, P_GN:P_GN + 384]
                pscale = lpk[:, P_PS:P_PS + 256]
                w2aug = lpk[0:33, P_W2:P_W2 + 256]

                NXS = 2
                xs = [sa(f"x{i}", [128, D], F32) for i in range(NXS)]
                r_xs = [Res(f"xs{i}") for i in range(NXS)]
                st_x = [tk.stream(f"a_x{i}") for i in range(NXS)]
                junk = sa("junk", [128, D], BF16)
                ssq = sa("ssq", [128, 8], F32)
                r_ssq = Res("ssq")
                hb = [sa(f"h{i}", [128, D], BF16) for i in range(2)]
                r_hb = [Res("h0"), Res("h1")]
                hT = [sa(f"hT{i}", [128, 8, 512], BF16) for i in range(2)]
                r_hT = [Res("hT0"), Res("hT1")]
                fmg = [sa(f"fmg{i}", [128, 4, 512], BF16) for i in range(2)]
                r_fmg = [Res("fmg0"), Res("fmg1")]
                gzs = [sa(f"gz{i}", [64, 512], F32) for i in range(2)]
                r_gzs = [Res("gz0"), Res("gz1")]
                fmq = sa("fmq", [128, 3, 512], BF16)
                fmk = sa("fmk", [128, 3, 512], BF16)
                fmqi = sa("fmqi", [128, 512], BF16)
                fmki = sa("fmki", [128, 512], BF16)
                r_fmq, r_fmk, r_fmqi, r_fmki = Res("fmq"), Res("fmk"), Res("fmqi"), Res("fmki")
                st_fm = [tk.stream(f"a_fm{i}") for i in range(4)]
                sg = sa("sg", [128, 1024], F32)
                r_sg = Res("sg")
                gg = sa("gg", [128, 384], F32)
                r_gg = Res("gg")
                psg = sa("psg", [128, 256], F32)
                r_psg = Res("psg")
                vgl = sa("vgl", [128, 384], BF16)
                r_vgl = Res("vgl")
                ktm = sa("ktm", [128, 256], F32)
                r_ktm = Res("ktm")
                pus = [sa(f"pu{i}", [128, 256], BF16) for i in range(2)]
                r_pus = [Res("pu0"), Res("pu1")]
                vaug = [sa(f"vaug{i}", [128, 6, 65], BF16) for i in range(2)]
                r_vaug = [Res("va0"), Res("va1")]
                st_va = [tk.stream(f"a_va{i}") for i in range(2)]
                ymx = [sa(f"ymx{i}", [128, 1024], BF16) for i in range(2)]
                r_ymx = [Res("ym0"), Res("ym1")]
                st_ym = [tk.stream(f"a_ym{i}") for i in range(2)]
                wis = [sa(f"wis{i}", [128, 4], F32) for i in range(2)]
                r_wis = [Res("wi0"), Res("wi1")]
                st_wi = [tk.stream(f"a_wi{i}") for i in range(2)]
                lt = sa("lt", [128, 256], F32)
                r_lt = Res("lt")
                ex = sa("ex", [128, 256], F32)
                r_ex = Res("ex")
                Eq = sa("Eq", [128, 256], F32)
                Ek = sa("Ek", [128, 256], F32)
                Ekd = sa("Ekd", [128, 256], F32)
                r_Eq, r_Ek, r_Ekd = Res("Eq"), Res("Ek"), Res("Ekd")
                qeT = sa("qeT", [128, 2, 128], BF16)
                keT = sa("keT", [128, 2, 128], BF16)
                kd = sa("kd", [128, 256], BF16)
                r_qeT, r_keT, r_kd = Res("qeT"), Res("keT"), Res("kd")
                ATs = sa("ATs", [128, 512], BF16)
                r_ATs = Res("ATs")
                S32 = sa("S32", [128, 2, 96], F32)
                S32m = sa("S32m", [128, 2, 96], F32)
                Sba = sa("Sba", [128, 2, 96], BF16)
                Sbb = sa("Sbb", [128, 2, 96], BF16)
                r_S32, r_S32m, r_Sba, r_Sbb = Res("S32"), Res("S32m"), Res("Sba"), Res("Sbb")
                rt4 = sa("rt4", [128, 8], F32)
                r_rt4 = Res("rt4")
                rstd4 = sa("rstd4", [128, 8], F32)
                r_rstd4 = Res("rstd4")
                pTs = sa("pTs", [64, 512], BF16)
                r_pTs = Res("pTs")

                tk.dve(lambda e: e.memset(S32[:], 0.0), W=[r_S32])
                tk.dve(lambda e: e.memset(Sba[:], 0.0), W=[r_Sba])
                tk.dve(lambda e: e.memset(S32m[:], 0.0), W=[r_S32m])
                tk.dve(lambda e: e.memset(Sbb[:], 0.0), W=[r_Sbb])
                for i in range(2):
                    tk.dve(lambda e, i=i: e.memset(gzs[i][:], 0.0), W=[r_gzs[i]])
                    tk.dve(lambda e, i=i: e.memset(gzs[i][32:33, :], 1.0), W=[r_gzs[i]])
                    tk.dve(lambda e, i=i: e.memset(vaug[i][:], 1.0), W=[r_vaug[i]])

                B_TR, B_FM, B_TM, B_G0, B_G1, B_G2 = 0, (1, 2), (3, 4), 5, 6, 7
                tr_bf = banks[B_TR].bitcast(BF16)

                def load_x(blk):
                    s = blk % NXS
                    tk.dma(st_x[s], xs[s][:], x_src[blk * 128:(blk + 1) * 128, :], R=[r_x[blk]], W=[r_xs[s]])

                for b0 in range(min(NXS, NT)):
                    load_x(b0)

                evac_rr = [0]

                def evac(out, in_, R, W):
                    evac_rr[0] += 1
                    if evac_rr[0] % 2:
                        tk.act(lambda e: e.copy(out=out, in_=in_), R=R, W=W)
                    else:
                        tk.dve(lambda e: e.tensor_copy(out=out, in_=in_), R=R, W=W)

                for tg in range(NG):
                    hs = tg % 2
                    for j in range(4):
                        blk = tg * 4 + j
                        s = blk % NXS
                        hh = blk % 2
                        tk.act(lambda e, s=s: e.activation(out=junk[:], in_=xs[s][:], func=AF.Square,
                                                            accum_out=ssq[:, 0:1]),
                               R=[r_xs[s]], W=[r_ssq])
                        tk.act(lambda e: e.activation(out=ssq[:, 1:2], in_=ssq[:, 0:1], func=AF.Sqrt,
                                                      bias=epst[:, 0:1], scale=1.0 / D),
                               R=[r_ssq, r_eps], W=[r_ssq])
                        tk.dve(lambda e: e.reciprocal(out=ssq[:, 2:3], in_=ssq[:, 1:2]), R=[r_ssq], W=[r_ssq])
                        tk.dve(lambda e, s=s, hh=hh: e.scalar_tensor_tensor(
                            out=hb[hh][:], in0=xs[s][:], scalar=ssq[:, 2:3], in1=normg,
                            op0=ALU.mult, op1=ALU.mult), R=[r_xs[s], r_ssq, r_lpk], W=[r_hb[hh]])
                        if blk + NXS < NT:
                            load_x(blk + NXS)
                        for k in range(8):
                            tk.pe(lambda e, k=k, hh=hh: e.transpose(
                                out=tr_bf[:, k * 128:(k + 1) * 128], in_=hb[hh][:, k * 128:(k + 1) * 128],
                                identity=IDb), R=[r_hb[hh], r_cbf], W=[rbank[B_TR]])
                        evac(hT[hs][:, :, j * 128:(j + 1) * 128],
                             tr_bf[:, :].rearrange("p (k c) -> p k c", k=8),
                             R=[rbank[B_TR]], W=[r_hT[hs]])
                    if stop_after == "norm":
                        raise _Stop()
                    for cb in range(13):
                        bk = B_FM[cb % 2]
                        M = 64 if cb == 4 else 128
                        for k in range(8):
                            tk.pe(lambda e, k=k, cb=cb, bk=bk, M=M: e.matmul(
                                out=banks[bk][0:M, :], lhsT=wfm[:, k, cb * 128:cb * 128 + M],
                                rhs=hT[hs][:, k, :], start=(k == 0), stop=(k == 7)),
                                R=[r_hT[hs], r_w], W=[rbank[bk]])
                        if cb < 4:
                            evac(fmg[hs][:, cb, :], banks[bk][:, :], R=[rbank[bk]], W=[r_fmg[hs]])
                        elif cb == 4:
                            evac(gzs[hs][0:32, :], banks[bk][0:32, :], R=[rbank[bk]], W=[r_gzs[hs]])
                        elif cb < 8:
                            evac(fmq[:, cb - 5, :], banks[bk][:, :], R=[rbank[bk]], W=[r_fmq])
                        elif cb < 11:
                            evac(fmk[:, cb - 8, :], banks[bk][:, :], R=[rbank[bk]], W=[r_fmk])
                        elif cb == 11:
                            evac(fmqi[:], banks[bk][:, :], R=[rbank[bk]], W=[r_fmqi])
                        else:
                            evac(fmki[:], banks[bk][:, :], R=[rbank[bk]], W=[r_fmki])
                    tsl = slice(tg * 512, (tg + 1) * 512)
                    tk.dma(st_fm[0], sqT_d[:, :, tsl], fmq[:], R=[r_fmq], W=[r_sq[tg]])
                    tk.dma(st_fm[1], skT_d[:, :, tsl], fmk[:], R=[r_fmk], W=[r_sk[tg]])
                    tk.dma(st_fm[2], sqi_d[0, :, tsl], fmqi[0:64, :], R=[r_fmqi], W=[r_sqi[tg]])
                    tk.dma(st_fm[2], sqi_d[1, :, tsl], fmqi[64:128, :], R=[r_fmqi], W=[r_sqi[tg]])
                    tk.dma(st_fm[3], ski_d[:, tsl], fmki[:], R=[r_fmki], W=[r_ski[tg]])

                    if stop_after == "fm":
                        raise _Stop()
                    for j in range(4):
                        blk = tg * 4 + j
                        s2 = blk % 2
                        jsl = slice(j * 128, (j + 1) * 128)

                        def tm_mm(gi, bk):
                            c0, cw = TM_GROUPS[gi]
                            for k in range(8):
                                tk.pe(lambda e, k=k: e.matmul(
                                    out=banks[bk][:, 0:cw], lhsT=hT[hs][:, k, jsl],
                                    rhs=wtm[:, k, c0:c0 + cw], start=(k == 0), stop=(k == 7)),
                                    R=[r_hT[hs], r_w], W=[rbank[bk]])

                        for gi in (0, 1):
                            bk = B_TM[gi]
                            tm_mm(gi, bk)
                            tk.act(lambda e, gi=gi, bk=bk: e.activation(
                                out=sg[:, gi * 512:(gi + 1) * 512], in_=banks[bk][:, :], func=AF.Silu),
                                R=[rbank[bk]], W=[r_sg])
                        if stop_after == "tm0":
                            raise _Stop()
                        bk = B_TM[0]
                        tm_mm(2, bk)
                        tk.dve(lambda e, bk=bk: e.tensor_copy(out=vgl[:], in_=banks[bk][:, 0:384]),
                               R=[rbank[bk]], W=[r_vgl])
                        tk.act(lambda e, bk=bk: e.copy(out=wis[s2][:], in_=banks[bk][:, 384:388]),
                               R=[rbank[bk]], W=[r_wis[s2]])
                        if "wi" not in _DIS:
                            tk.dma(st_wi[s2], swi_d[blk * 128:(blk + 1) * 128, :], wis[s2][:], R=[r_wis[s2]], W=[r_swi[blk]])
                        if stop_after == "tm1":
                            raise _Stop()
                        bk = B_TM[1]
                        if "mm3" not in _DIS:
                            tm_mm(3, bk)
                        if "ktm" not in _DIS:
                            tk.act(lambda e, bk=bk: e.copy(out=ktm[:], in_=banks[bk][:, 0:256]),
                                   R=[rbank[bk]], W=[r_ktm])
                        if "pus" not in _DIS:
                            tk.dve(lambda e, bk=bk: e.tensor_copy(out=pus[s2][:], in_=banks[bk][:, 256:512]),
                                   R=[rbank[bk]], W=[r_pus[s2]])
                        if stop_after == "tm2":
                            raise _Stop()
                        bk = B_TM[0]
                        tm_mm(4, bk)
                        tk.act(lambda e, bk=bk: e.copy(
                            out=vaug[s2][:, :, 0:64],
                            in_=banks[bk][:, 0:384].rearrange("p (h d) -> p h d", h=6)),
                            R=[rbank[bk]], W=[r_vaug[s2]])
                        if "va" not in _DIS:
                            tk.dma(st_va[s2], sv_d[blk * 128:(blk + 1) * 128, :],
                                   vaug[s2][:].rearrange("p h d -> p (h d)"), R=[r_vaug[s2]], W=[r_sv[blk]])
                        tk.op(GPE, lambda e: e.tensor_copy(out=ymx[s2][:, 384:768], in_=sg[:, 384:768]),
                                R=[r_sg], W=[r_ymx[s2]])
                        tk.op(GPE, lambda e: e.tensor_tensor(out=gg[:], in0=sg[:, 0:384], in1=gnorm4, op=ALU.mult),
                                R=[r_sg, r_lpk], W=[r_gg])
                        tk.op(GPE, lambda e: e.tensor_tensor(out=psg[:], in0=sg[:, 768:1024], in1=pscale, op=ALU.mult),
                                R=[r_sg, r_lpk], W=[r_psg])

                        if stop_after == "tm":
                            raise _Stop()
                        tk.pe(lambda e: e.matmul(out=banks[B_G0][:, 0:256], lhsT=gzs[hs][0:33, jsl],
                                                 rhs=w2aug, start=True, stop=True),
                              R=[r_gzs[hs], r_lpk], W=[rbank[B_G0]])
                        tk.act(lambda e: e.activation(out=ex[:], in_=banks[B_G0][:, 0:256], func=AF.Exp, scale=-1.0),
                               R=[rbank[B_G0]], W=[r_ex])
                        tk.act(lambda e: e.activation(out=lt[:], in_=ex[:], func=AF.Ln, bias=1.0),
                               R=[r_ex], W=[r_lt])
                        for p in range(2):
                            tk.pe(lambda e, p=p: e.matmul(
                                out=banks[B_G0][:, p * 128:(p + 1) * 128], lhsT=lt[:, p * 128:(p + 1) * 128],
                                rhs=cst[:, C_TRI:C_TRI + 128], start=True, stop=True),
                                R=[r_lt, r_cst], W=[rbank[B_G0]])
                        tk.pe(lambda e: e.matmul(out=banks[B_G0][:, 256:512], lhsT=cst[:, C_UT:C_UT + 128],
                                                 rhs=lt[:], start=True, stop=True),
                              R=[r_lt, r_cst], W=[rbank[B_G0]])
                        tk.act(lambda e: e.activation(out=Eq[:], in_=banks[B_G0][:, 0:256], func=AF.Exp),
                               R=[rbank[B_G0]], W=[r_Eq])
                        tk.act(lambda e: e.activation(out=Ek[:], in_=banks[B_G0][:, 0:256], func=AF.Exp, scale=-1.0),
                               R=[rbank[B_G0]], W=[r_Ek])
                        tk.act(lambda e: e.activation(out=Ekd[:], in_=banks[B_G0][:, 256:512], func=AF.Exp),
                               R=[rbank[B_G0]], W=[r_Ekd])
                        tk.dve(lambda e: e.scalar_tensor_tensor(
                            out=qeT[:], in0=fmg[hs][:, 0:2, jsl], scalar=48.0 ** -0.5,
                            in1=Eq[:].rearrange("p (a t) -> p a t", a=2), op0=ALU.mult, op1=ALU.mult),
                            R=[r_fmg[hs], r_Eq], W=[r_qeT])
                        tk.dve(lambda e: e.tensor_tensor(
                            out=keT[:], in0=fmg[hs][:, 2:4, jsl],
                            in1=Ek[:].rearrange("p (a t) -> p a t", a=2), op=ALU.mult),
                            R=[r_fmg[hs], r_Ek], W=[r_keT])
                        tk.dve(lambda e: e.tensor_tensor(out=kd[:], in0=ktm[:], in1=Ekd[:], op=ALU.mult),
                               R=[r_ktm, r_Ekd], W=[r_kd])

                        def u_mm(c):
                            for h in range(4):
                                p, base = h // 2, 64 * (h % 2)
                                tk.pe(lambda e, h=h, p=p, base=base: e.matmul(
                                    out=banks[B_G0][base:base + 64, c * 192 + p * 96:c * 192 + (p + 1) * 96],
                                    lhsT=kd[c * 64:(c + 1) * 64, h * 64:(h + 1) * 64],
                                    rhs=vgl[c * 64:(c + 1) * 64, h * 96:(h + 1) * 96], start=True, stop=True),
                                    R=[r_kd, r_vgl], W=[rbank[B_G0]])

                        u_mm(0)
                        for p in range(2):
                            tk.dve(lambda e, p=p: e.scalar_tensor_tensor(
                                out=S32m[:, p, :], in0=S32[:, p, :], scalar=Eq[:, p * 128 + 63:p * 128 + 64],
                                in1=banks[B_G0][:, p * 96:(p + 1) * 96], op0=ALU.mult, op1=ALU.add),
                                R=[r_S32, r_Eq, rbank[B_G0]], W=[r_S32m])
                        tk.op(GPE, lambda e: e.tensor_copy(out=Sbb[:], in_=S32m[:]), R=[r_S32m], W=[r_Sbb])
                        for h in range(4):
                            p, base = h // 2, 64 * (h % 2)
                            tk.pe(lambda e, h=h, p=p, base=base: e.matmul(
                                out=banks[B_G1][:, h * 128:(h + 1) * 128], lhsT=keT[base:base + 48, p, :],
                                rhs=qeT[base:base + 48, p, :], start=True, stop=True),
                                R=[r_keT, r_qeT], W=[rbank[B_G1]], serial=(h > 0))
                        tk.dve(lambda e: e.tensor_tensor(out=ATs[:], in0=banks[B_G1][:, :],
                                                         in1=cst[:, C_MASK4:C_MASK4 + 512], op=ALU.mult),
                               R=[rbank[B_G1], r_cst], W=[r_ATs])
                        for h in range(4):
                            p, base = h // 2, 64 * (h % 2)
                            osl = slice(h * 96, (h + 1) * 96)
                            tk.pe(lambda e, h=h, osl=osl: e.matmul(
                                out=banks[B_G2][:, osl], lhsT=ATs[:, h * 128:(h + 1) * 128],
                                rhs=vgl[:, osl], start=True, stop=False),
                                R=[r_ATs, r_vgl], W=[rbank[B_G2]])
                            tk.pe(lambda e, p=p, base=base, osl=osl: e.matmul(
                                out=banks[B_G2][0:64, osl], lhsT=qeT[base:base + 48, p, 0:64],
                                rhs=Sba[base:base + 48, p, :], start=False, stop=True),
                                R=[r_qeT, r_Sba], W=[rbank[B_G2]])
                            tk.pe(lambda e, p=p, base=base, osl=osl: e.matmul(
                                out=banks[B_G2][64:128, osl], lhsT=qeT[base:base + 48, p, 64:128],
                                rhs=Sbb[base:base + 48, p, :], start=False, stop=True),
                                R=[r_qeT, r_Sbb], W=[rbank[B_G2]])
                        u_mm(1)
                        for p in range(2):
                            tk.dve(lambda e, p=p: e.scalar_tensor_tensor(
                                out=S32[:, p, :], in0=S32m[:, p, :], scalar=Eq[:, p * 128 + 127:p * 128 + 128],
                                in1=banks[B_G0][:, 192 + p * 96:192 + (p + 1) * 96], op0=ALU.mult, op1=ALU.add),
                                R=[r_S32m, r_Eq, rbank[B_G0]], W=[r_S32])
                        tk.op(GPE, lambda e: e.tensor_copy(out=Sba[:], in_=S32[:]), R=[r_S32], W=[r_Sba])
                        for h in range(4):
                            tk.act(lambda e, h=h: e.activation(
                                out=junk[:, 0:96], in_=banks[B_G2][:, h * 96:(h + 1) * 96], func=AF.Square,
                                accum_out=rt4[:, h:h + 1]), R=[rbank[B_G2]], W=[r_rt4])
                        tk.act(lambda e: e.activation(out=rt4[:, 4:8], in_=rt4[:, 0:4], func=AF.Sqrt,
                                                      bias=epst[:, 0:1], scale=1.0 / 96.0),
                               R=[r_rt4, r_eps], W=[r_rt4])
                        tk.dve(lambda e: e.reciprocal(out=rstd4[:, 0:4], in_=rt4[:, 4:8]), R=[r_rt4], W=[r_rstd4])
                        for h in range(4):
                            tk.dve(lambda e, h=h: e.scalar_tensor_tensor(
                                out=ymx[s2][:, h * 96:(h + 1) * 96], in0=banks[B_G2][:, h * 96:(h + 1) * 96],
                                scalar=rstd4[:, h:h + 1], in1=gg[:, h * 96:(h + 1) * 96],
                                op0=ALU.mult, op1=ALU.mult),
                                R=[rbank[B_G2], r_rstd4, r_gg], W=[r_ymx[s2]])

                        if stop_after == "gla":
                            raise _Stop()
                        md = MD0b if blk == 0 else MDb
                        for g in range(4):
                            tk.pe(lambda e, g=g: e.matmul(
                                out=banks[B_G1][0:64, g * 128:(g + 1) * 128], lhsT=pus[s2][:, g * 64:(g + 1) * 64],
                                rhs=md[:, g * 128:(g + 1) * 128], start=True, stop=(blk == 0)),
                                R=[r_pus[s2], r_cbf], W=[rbank[B_G1]])
                            if blk > 0:
                                tk.pe(lambda e, g=g: e.matmul(
                                    out=banks[B_G1][0:64, g * 128:(g + 1) * 128],
                                    lhsT=pus[1 - s2][:, g * 64:(g + 1) * 64],
                                    rhs=MPb[:, g * 128:(g + 1) * 128], start=False, stop=True),
                                    R=[r_pus[1 - s2], r_cbf], W=[rbank[B_G1]])
                        tk.act(lambda e: e.copy(out=pTs[:], in_=banks[B_G1][0:64, :]), R=[rbank[B_G1]], W=[r_pTs])
                        for g in range(4):
                            tk.pe(lambda e, g=g: e.matmul(
                                out=banks[B_G1][:, g * 64:(g + 1) * 64], lhsT=pTs[0:64, g * 128:(g + 1) * 128],
                                rhs=pwb[0:64, g * 64:(g + 1) * 64], start=True, stop=True),
                                R=[r_pTs, r_pwb], W=[rbank[B_G1]])
                        tk.dve(lambda e: e.tensor_tensor(out=ymx[s2][:, 768:1024], in0=banks[B_G1][:, 0:256],
                                                         in1=psg[:], op=ALU.mult),
                               R=[rbank[B_G1], r_psg], W=[r_ymx[s2]])
                        tk.dma(st_ym[s2], sym_d[blk * 128:(blk + 1) * 128, :], ymx[s2][:], R=[r_ymx[s2]], W=[r_sym[blk]])
                tk.barrier()

            with ExitStack() as pa:
                try:
                    phase_a(pa)
                except _Stop:
                    stopped[0] = True
            if stopped[0]:
                return
            def phase_b1(pb):
                def sbt(name, shape, dt):
                    return pb.enter_context(nc.sbuf_tensor(f"b{l}_{name}", shape, dt))

                ki4 = sbt("ki4", [128, T], BF16)
                r_ki4 = Res("ki4")
                scores = [sbt(f"scores{i}", [128, T], F32) for i in range(2)]
                r_sc = [Res("scores0"), Res("scores1")]
                junkb = sbt("junkb", [128, T], BF16)
                maskb = sbt("maskb", [128, T], BF16)
                r_mask = Res("maskb")
                mst = [sbt(f"mst{i}", [128, NT, 128], BF16) for i in range(2)]
                r_mst = [Res("mst0"), Res("mst1")]
                st_mst = [tk.stream(f"b_mst{i}") for i in range(2)]
                qis = [sbt(f"qis{i}", [64, 2, 128], BF16) for i in range(2)]
                r_qis = [Res("qis0"), Res("qis1")]
                st_qi = [tk.stream(f"b_qi{i}") for i in range(2)]
                wib = [sbt(f"wib{i}", [128, 4], F32) for i in range(2)]
                r_wib = [Res("wib0"), Res("wib1")]
                st_wib = [tk.stream(f"b_wib{i}") for i in range(2)]
                rh = [sbt(f"rh{i}", [128, 4, 512], BF16) for i in range(2)]
                r_rh = [Res("rh0"), Res("rh1")]
                dgw = [sbt(f"dgw{i}", [128, 4, 128], BF16) for i in range(2)]
                r_dgw = [Res("dgw0"), Res("dgw1")]
                bis = sbt("bis", [128, 8], F32)
                r_bis = Res("bis")
                bw = sbt("bw", [128, 32], F32)
                r_bw = Res("bw")
                st_k = tk.stream(f"b_ki")
                nch = max(1, T // 2048)
                cwk = T // nch
                for c in range(nch):
                    tk.dma(st_k, ki4[:, c * cwk:(c + 1) * cwk], ski_d[:, c * cwk:(c + 1) * cwk],
                           R=r_ski, W=[r_ki4])
                trb = [banks[6].bitcast(BF16), banks[7].bitcast(BF16)]
                rr = [0, 0]
                r_mid, r_cd, r_ca, r_lo, r_g, r_M = (Res("mid"), Res("cd"), Res("ca"), Res("lo"), Res("g"), Res("M"))
                r_thr2 = Res("thr2")
                ACT_SHARE = 0.5

                def gen_scores(qb):
                    s = qb % 2
                    S = (qb + 1) * 128
                    g = qb // 4
                    qsl = slice(qb * 128, (qb + 1) * 128)
                    tk.dma(st_qi[s], qis[s][:], sqi_d[:, :, qsl].rearrange("a p t -> p a t"), R=[r_sqi[g]], W=[r_qis[s]])
                    tk.dma(st_wib[s], wib[s][:], swi_d[qsl, :], R=[r_swi[qb]], W=[r_wib[s]])
                    for h in range(4):
                        tk.op(GPE, lambda e, h=h: e.tensor_scalar(
                            out=dgw[s][:, h, :], in0=cst[:, C_ID:C_ID + 128], scalar1=wib[s][:, h:h + 1],
                            scalar2=None, op0=ALU.mult), R=[r_cst, r_wib[s]], W=[r_dgw[s]])
                    nkt = (S + 511) // 512
                    gt0 = rr[0]
                    rr[0] += nkt

                    def kw_(kt):
                        return min(512, S - kt * 512)

                    def score_mm(kt):
                        for h in range(4):
                            tk.pe(lambda e, h=h: e.matmul(
                                out=banks[h][:, 0:kw_(kt)], lhsT=qis[s][32 * (h % 2):32 * (h % 2) + 32, h // 2, :],
                                rhs=ki4[32 * (h % 2):32 * (h % 2) + 32, kt * 512:kt * 512 + kw_(kt)],
                                start=True, stop=True), R=[r_qis[s], r_ki4], W=[rbank[h]])

                    def relu(kt):
                        r = (gt0 + kt) % 2
                        for h in range(4):
                            tk.act(lambda e, h=h: e.activation(
                                out=rh[r][:, h, 0:kw_(kt)], in_=banks[h][:, 0:kw_(kt)], func=AF.Relu,
                                scale=(32.0 ** -0.5) * 0.5), R=[rbank[h]], W=[r_rh[r]])

                    def diag_mm(kt):
                        r = (gt0 + kt) % 2
                        bk = 4 + r
                        for h in range(4):
                            tk.pe(lambda e, h=h: e.matmul(
                                out=banks[bk][:, 0:kw_(kt)], lhsT=dgw[s][:, h, :], rhs=rh[r][:, h, 0:kw_(kt)],
                                start=(h == 0), stop=(h == 3)), R=[r_dgw[s], r_rh[r]], W=[rbank[bk]])

                    def copy(kt):
                        bk = 4 + (gt0 + kt) % 2
                        tk.act(lambda e: e.copy(out=scores[s][:, kt * 512:kt * 512 + kw_(kt)],
                                                in_=banks[bk][:, 0:kw_(kt)]), R=[rbank[bk]], W=[r_sc[s]])

                    for t in range(nkt + 2):
                        if 0 <= t - 1 < nkt:
                            relu(t - 1)
                        if t < nkt:
                            score_mm(t)
                        if 0 <= t - 1 < nkt:
                            diag_mm(t - 1)
                        if 0 <= t - 2 < nkt:
                            copy(t - 2)
                        yield

                def select(qb, filler):
                    s = qb % 2
                    S = (qb + 1) * 128
                    g, j = qb // 4, qb % 4
                    qsl = slice(qb * 128, (qb + 1) * 128)
                    sc = scores[s]
                    rs = r_sc[s]
                    if qb >= QB0:
                        tk.dve(lambda e: e.tensor_reduce(out=bis[:, 0:1], in_=sc[:, 0:S], axis=AX.X, op=ALU.max,
                                                         apply_absolute_value=True), R=[rs], W=[r_M])
                        tk.dve(lambda e: e.tensor_scalar(out=bw[:, 0:N_BISECT], in0=cst[:, C_POW2:C_POW2 + N_BISECT],
                                                         scalar1=bis[:, 0:1], scalar2=2.0001, op0=ALU.mult, op1=ALU.mult),
                               R=[r_cst, r_M], W=[r_bw])
                        tk.dve(lambda e: e.tensor_scalar(out=bis[:, 1:2], in0=bis[:, 0:1], scalar1=-1.0, scalar2=None,
                                                         op0=ALU.mult), R=[r_M], W=[r_lo])
                    else:
                        tk.dve(lambda e: e.memset(bis[:, 1:2], -1.0e29), W=[r_lo])
                    tk.dve(lambda e: e.tensor_tensor(out=sc[:, qsl], in0=sc[:, qsl],
                                                     in1=cst[:, C_NEGM:C_NEGM + 128], op=ALU.add),
                           R=[rs, r_cst], W=[rs])
                    if qb >= QB0:
                        if qb + 1 < NT:
                            nkt_n = ((qb + 2) * 128 + 511) // 512
                            fsteps = [(3.0 if 1 <= t <= nkt_n else 0.0) + (0.6 if 2 <= t <= nkt_n + 1 else 0.0)
                                      for t in range(nkt_n + 2)]
                        else:
                            fsteps = []

                        def split(k):
                            fk = fsteps[k - 1] if 1 <= k <= len(fsteps) else 0.0
                            a_k = (S * 1.045e-3 + 0.25 - fk) / (S * 1.705e-3)
                            a_k = min(0.72, max(0.08, a_k))
                            s1 = min(S, max(128, int(round(S * (1.0 - a_k) / 128.0)) * 128))
                            return s1, S - s1, float(topk) - 0.5 - (S - s1) / 2.0
                        tk.dve(lambda e: e.tensor_tensor(out=bis[:, 2:3], in0=bis[:, 1:2], in1=bw[:, 0:1], op=ALU.add),
                               R=[r_lo, r_bw], W=[r_mid])
                        for k in range(N_BISECT):
                            lastk = (k == N_BISECT - 1)
                            S1, nA, thr = split(k)
                            if nA > 0:
                                tk.act(lambda e: e.activation(out=junkb[:, S1:S], in_=sc[:, S1:S], func=AF.Sign,
                                                              bias=bis[:, 2:3], scale=-1.0, accum_out=bis[:, 5:6]),
                                       R=[rs, r_mid], W=[r_ca])
                            tk.dve(lambda e: e.tensor_scalar(out=junkb[:, 0:S1], in0=sc[:, 0:S1], scalar1=bis[:, 2:3],
                                                             scalar2=None, op0=ALU.is_ge, op1=ALU.add,
                                                             accum_out=bis[:, 3:4]), R=[rs, r_mid], W=[r_cd])
                            if nA > 0:
                                tk.act(lambda e: e.activation(out=bis[:, 6:7], in_=bis[:, 5:6], func=AF.Identity,
                                                              bias=thr, scale=0.5), R=[r_ca], W=[r_thr2])
                                tk.dve(lambda e, k=k: e.tensor_scalar(out=bis[:, 4:5], in0=bis[:, 3:4], scalar1=bis[:, 6:7],
                                                                      scalar2=bw[:, k:k + 1], op0=ALU.is_ge, op1=ALU.mult),
                                       R=[r_cd, r_bw, r_thr2], W=[r_g])
                            else:
                                tk.dve(lambda e, k=k: e.tensor_scalar(out=bis[:, 4:5], in0=bis[:, 3:4], scalar1=thr,
                                                                      scalar2=bw[:, k:k + 1], op0=ALU.is_ge, op1=ALU.mult),
                                       R=[r_cd, r_bw], W=[r_g])
                            if not lastk:
                                tk.dve(lambda e, k=k: e.scalar_tensor_tensor(
                                    out=bis[:, 2:3], in0=bis[:, 2:3], scalar=bw[:, k + 1:k + 2], in1=bis[:, 4:5],
                                    op0=ALU.subtract, op1=ALU.add), R=[r_mid, r_bw, r_g], W=[r_mid])
                            else:
                                tk.dve(lambda e, k=k: e.scalar_tensor_tensor(
                                    out=bis[:, 1:2], in0=bis[:, 2:3], scalar=bw[:, k:k + 1], in1=bis[:, 4:5],
                                    op0=ALU.subtract, op1=ALU.add), R=[r_mid, r_bw, r_g], W=[r_lo])
                            if filler is not None:
                                next(filler, None)
                    if filler is not None:
                        for _ in filler:
                            pass
                    tk.dve(lambda e: e.tensor_scalar(out=maskb[:, 0:S], in0=sc[:, 0:S], scalar1=bis[:, 1:2],
                                                     scalar2=None, op0=ALU.is_ge), R=[rs, r_lo], W=[r_mask])
                    kb0 = 0
                    while kb0 <= qb:
                        n = min(8, qb + 1 - kb0)
                        t = rr[1] % 2
                        rr[1] += 1
                        for i in range(n):
                            tk.pe(lambda e, i=i: e.transpose(out=trb[t][:, i * 128:(i + 1) * 128],
                                                             in_=maskb[:, (kb0 + i) * 128:(kb0 + i + 1) * 128],
                                                             identity=IDb), R=[r_mask, r_cbf], W=[rbank[6 + t]])
                        tk.act(lambda e: e.copy(out=mst[s][:, kb0:kb0 + n, :],
                                                in_=trb[t][:, 0:n * 128].rearrange("p (k c) -> p k c", k=n)),
                               R=[rbank[6 + t]], W=[r_mst[s]])
                        kb0 += n
                    nkbg = 4 * g + 4
                    if qb + 1 < nkbg:
                        tk.op(GPE, lambda e: e.memset(mst[s][:, qb + 1:nkbg, :], 0.0), W=[r_mst[s]])
                    for k0 in range(0, nkbg, 8):
                        k1 = min(nkbg, k0 + 8)
                        tk.dma(st_mst[s], smk_d[g, k0:k1, :, j * 128:(j + 1) * 128].rearrange("k p q -> p k q"),
                               mst[s][:, k0:k1, :], R=[r_mst[s]], W=[r_smk[qb]])

                for _ in gen_scores(0):
                    pass
                for qb in range(NT):
                    select(qb, gen_scores(qb + 1) if qb + 1 < NT else None)
                tk.barrier()

            def phase_b2(pb):
                def sbt(name, shape, dt):
                    return pb.enter_context(nc.sbuf_tensor(f"c{l}_{name}", shape, dt))

                kT = sbt("kT", [128, 3, T], BF16)
                r_kT = Res("kT")
                vA = sbt("vA", [128, NT, 390], BF16)
                r_vA = Res("vA")
                wout = sbt("wout", [128, 8, D], BF16)
                r_wo = Res("wout")
                st_kv = [tk.stream(f"c_kv{i}") for i in range(2)]
                for p in range(3):
                    tk.dma(st_kv[0], kT[:, p, :], skT_d[:, p, :], R=r_sk, W=[r_kT])
                vsrc = sv_d.rearrange("(b p) c -> p b c", p=128)
                bstep = 8
                for b0 in range(0, NT, bstep):
                    b1 = min(NT, b0 + bstep)
                    tk.dma(st_kv[1], vA[:, b0:b1, :], vsrc[:, b0:b1, :], R=r_sv, W=[r_vA])
                with ExitStack() as ws:
                    wst = [ws.enter_context(nc.sbuf_tensor(f"c{l}_wst{i}", [128, 8, 256], F32)) for i in range(2)]
                    r_wst = [Res("wst0"), Res("wst1")]
                    srcv = wout_d[l].rearrange("(k p) c -> p k c", p=128)
                    for ci in range(4):
                        c0 = ci * 256
                        s = ci % 2
                        tk.dma(st_misc[1 + s], wst[s][:], srcv[:, :, c0:c0 + 256], W=[r_wst[s]])
                        if ci % 2:
                            tk.act(lambda e, s=s, c0=c0: e.copy(out=wout[:, :, c0:c0 + 256], in_=wst[s][:]),
                                   R=[r_wst[s]], W=[r_wo])
                        else:
                            tk.dve(lambda e, s=s, c0=c0: e.tensor_copy(out=wout[:, :, c0:c0 + 256], in_=wst[s][:]),
                                   R=[r_wst[s]], W=[r_wo])
                    tk.barrier()
                r_wo = Res("wout2")
                qTg = [sbt(f"qTg{i}", [128, 3, 512], BF16) for i in range(2)]
                r_qTg = [Res("qTg0"), Res("qTg1")]
                st_q = [tk.stream(f"c_q{i}") for i in range(2)]
                NMP = 2
                mp = [sbt(f"mp{i}", [128, 4, 512], BF16) for i in range(NMP)]
                r_mp = [Res(f"mp{i}") for i in range(NMP)]
                st_mp = [tk.stream(f"c_mp{i}") for i in range(NMP)]
                NE = 4
                esb = [sbt(f"esb{i}", [128, 512], BF16) for i in range(NE)]
                r_esb = [Res(f"esb{i}") for i in range(NE)]
                pmb = [sbt(f"pmb{i}", [128, 512], BF16) for i in range(NE)]
                r_pmb = [Res(f"pmb{i}") for i in range(NE)]
                osb = sbt("osb", [65, 6, 512], F32)
                r_osb = Res("osb")
                rinv = sbt("rinv", [128, 8], F32)
                r_rinv = Res("rinv")
                ymx = [sbt(f"ymx{i}", [128, D], BF16) for i in range(2)]
                r_ymx = [Res("ymx0"), Res("ymx1")]
                st_ym = [tk.stream(f"c_ym{i}") for i in range(2)]
                yT = sbt("yT", [128, 8, 128], BF16)
                r_yT = Res("yT")
                xt = [sbt(f"xt{i}", [128, D], F32) for i in range(2)]
                r_xt = [Res("xt0"), Res("xt1")]
                st_xl = [tk.stream(f"c_xl{i}") for i in range(2)]
                st_xs = [tk.stream(f"c_xs{i}") for i in range(2)]
                fst = sbt("fst", [128, 8], F32)
                r_fst = Res("fst")
                QK = (6, 7)
                qkbf = banks[7].bitcast(BF16)
                cnt = [0, 0]
                pend = []
                for g in range(NG):
                    gs = g % 2
                    nkb = 4 * g + 4
                    tk.dma(st_q[gs], qTg[gs][:], sqT_d[:, :, g * 512:(g + 1) * 512], R=[r_sq[g]], W=[r_qTg[gs]])
                    for kc in range(g + 1):
                        ms = cnt[1] % NMP
                        cnt[1] += 1
                        tk.dma(st_mp[ms], mp[ms][:], smk_d[g, kc * 4:(kc + 1) * 4, :, :].rearrange("k p q -> p k q"),
                               R=r_smk[4 * g:4 * g + 4], W=[r_mp[ms]])
                        for kbi in range(4):
                            kb = kc * 4 + kbi
                            for p in range(3):
                                slots = []
                                for hh in range(2):
                                    h, base = 2 * p + hh, 64 * hh
                                    i = cnt[0]
                                    cnt[0] += 1
                                    qb_ = QK[hh]
                                    es_ = i % NE
                                    slots.append((h, qb_, es_))
                                    tk.pe(lambda e, base=base, qb_=qb_: e.matmul(
                                        out=banks[qb_][:, :], lhsT=kT[base:base + 64, p, kb * 128:(kb + 1) * 128],
                                        rhs=qTg[gs][base:base + 64, p, :], start=True, stop=True),
                                        R=[r_kT, r_qTg[gs]], W=[rbank[qb_]])
                                for (h, qb_, es_) in slots:
                                    for j in range(4):
                                        d = 4 * g + j - kb
                                        if d == 0 or d == 1:
                                            off = G_BD if d == 0 else G_BP
                                            tk.dve(lambda e, j=j, off=off, h=h, qb_=qb_: e.tensor_tensor(
                                                out=banks[qb_][:, j * 128:(j + 1) * 128],
                                                in0=banks[qb_][:, j * 128:(j + 1) * 128],
                                                in1=gpk[:, off + h * 128:off + (h + 1) * 128], op=ALU.add),
                                                R=[r_gpk], W=[rbank[qb_]])
                                    tk.act(lambda e, h=h, qb_=qb_, es_=es_: e.activation(
                                        out=esb[es_][:], in_=banks[qb_][:, :], func=AF.Exp,
                                        bias=gpk[:, G_C31 + h:G_C31 + h + 1], scale=0.125),
                                        R=[rbank[qb_], r_gpk], W=[r_esb[es_]])
                                    tk.dve(lambda e, es_=es_: e.tensor_tensor(out=pmb[es_][:], in0=esb[es_][:],
                                                                              in1=mp[ms][:, kbi, :], op=ALU.mult),
                                           R=[r_esb[es_], r_mp[ms]], W=[r_pmb[es_]])

                                    def pv(h=h, kb=kb, es_=es_):
                                        tk.pe(lambda e: e.matmul(
                                            out=banks[h][0:65, :], lhsT=vA[:, kb, h * 65:(h + 1) * 65], rhs=pmb[es_][:],
                                            start=(kb == 0), stop=(kb == nkb - 1)),
                                            R=[r_vA, r_pmb[es_]], W=[rbank[h]])
                                    pend.append(pv)
                                while len(pend) > 2:
                                    pend.pop(0)()
                    while pend:
                        pend.pop(0)()
                    for h in range(6):
                        if h % 2:
                            tk.act(lambda e, h=h: e.copy(out=osb[0:65, h, :], in_=banks[h][0:65, :]),
                                   R=[rbank[h]], W=[r_osb])
                        else:
                            tk.dve(lambda e, h=h: e.tensor_copy(out=osb[0:65, h, :], in_=banks[h][0:65, :]),
                                   R=[rbank[h]], W=[r_osb])
                    for j in range(4):
                        qb = 4 * g + j
                        s = qb % 2
                        jsl = slice(j * 128, (j + 1) * 128)
                        rsl = slice(qb * 128, (qb + 1) * 128)
                        tk.dma(st_ym[s], ymx[s][:], sym_d[rsl, :], R=[r_sym[qb]], W=[r_ymx[s]])
                        tk.dma(st_xl[s], xt[s][:], x_src[rsl, :], R=[r_x[qb]], W=[r_xt[s]])
                        for h in range(6):
                            tk.pe(lambda e, h=h: e.transpose(out=banks[6][:, h * 65:(h + 1) * 65], in_=osb[0:65, h, jsl],
                                                             identity=cst[0:65, C_ID:C_ID + 65]),
                                  R=[r_osb, r_cst], W=[rbank[6]])
                        tk.dve(lambda e: e.reciprocal(
                            out=rinv[:, 0:6],
                            in_=banks[6][:, 0:390].rearrange("p (h d) -> p h d", d=65)[:, :, 64]),
                            R=[rbank[6]], W=[r_rinv])
                        for h in range(6):
                            ysl = slice(384 + h * 64, 384 + (h + 1) * 64)
                            tk.dve(lambda e, h=h, ysl=ysl: e.scalar_tensor_tensor(
                                out=ymx[s][:, ysl], in0=banks[6][:, h * 65:h * 65 + 64], scalar=rinv[:, h:h + 1],
                                in1=ymx[s][:, ysl], op0=ALU.mult, op1=ALU.mult),
                                R=[rbank[6], r_rinv, r_ymx[s]], W=[r_ymx[s]])
                        for k in range(8):
                            tk.pe(lambda e, k=k: e.transpose(out=qkbf[:, k * 128:(k + 1) * 128],
                                                             in_=ymx[s][:, k * 128:(k + 1) * 128], identity=IDb),
                                  R=[r_ymx[s], r_cbf], W=[rbank[7]])
                        tk.act(lambda e: e.copy(out=yT[:], in_=qkbf[:, :].rearrange("p (k c) -> p k c", k=8)),
                               R=[rbank[7]], W=[r_yT])
                        for n in range(2):
                            for k in range(8):
                                tk.pe(lambda e, n=n, k=k: e.matmul(
                                    out=banks[n][:, :], lhsT=yT[:, k, :], rhs=wout[:, k, n * 512:(n + 1) * 512],
                                    start=(k == 0), stop=(k == 7)), R=[r_yT, r_wo], W=[rbank[n]])
                            tk.dve(lambda e, n=n: e.tensor_tensor(
                                out=xt[s][:, n * 512:(n + 1) * 512], in0=xt[s][:, n * 512:(n + 1) * 512],
                                in1=banks[n][:, :], op=ALU.add), R=[rbank[n], r_xt[s]], W=[r_xt[s]])
                        if not last:
                            tk.dma(st_xs[s], xres_d[rsl, :], xt[s][:], R=[r_xt[s]], W=[r_x[qb]])
                        else:
                            tk.act(lambda e: e.activation(out=yT[:].rearrange("p k c -> p (k c)"), in_=xt[s][:],
                                                          func=AF.Square, accum_out=fst[:, 0:1]),
                                   R=[r_xt[s]], W=[r_fst, r_yT])
                            tk.act(lambda e: e.activation(out=fst[:, 1:2], in_=fst[:, 0:1], func=AF.Sqrt,
                                                          bias=epst[:, 0:1], scale=1.0 / D),
                                   R=[r_fst, r_eps], W=[r_fst])
                            tk.dve(lambda e: e.reciprocal(out=fst[:, 2:3], in_=fst[:, 1:2]), R=[r_fst], W=[r_fst])
                            tk.dve(lambda e: e.scalar_tensor_tensor(
                                out=xt[s][:], in0=xt[s][:], scalar=fst[:, 2:3], in1=gpk[:, G_FN:G_FN + D],
                                op0=ALU.mult, op1=ALU.mult), R=[r_xt[s], r_fst, r_gpk], W=[r_xt[s]])
                            tk.dma(st_xs[s], y_d[rsl, :], xt[s][:], R=[r_xt[s]], W=[r_x[qb]])
                tk.barrier()

            if stop_after == "A":
                stopped[0] = True
                return
            with ExitStack() as pb:
                phase_b1(pb)
            if stop_after == "B1":
                stopped[0] = True
                return
            with ExitStack() as pb:
                phase_b2(pb)

        stopped = [False]
        for l in range(depth if stop_after != "init" else 0):
            layer(l)
            if stopped[0]:
                break

        tk.barrier()
    print("instructions emitted:", tk.ninst)
    return nc


def prep_inputs(x, norm_g, w_in, gla_gate_w2, gla_gate_b, gla_norm_g, rel_bias, pool_w, pool_scale,
                w_out, final_norm_g, depth=DEPTH):
    f = np.float32
    w_in = np.asarray(w_in, f)
    wfm = np.zeros((depth, D, NFM), f)
    wtm = np.zeros((depth, D, NTM), f)
    lp = np.zeros((depth, 128, NPL), f)
    for l in range(depth):
        wfm[l], wtm[l] = _w_layouts(w_in[l])
        lp[l] = _layer_pack(np.asarray(norm_g[l], f), np.asarray(gla_norm_g[l], f),
                            np.asarray(pool_scale[l], f), np.asarray(gla_gate_w2[l], f),
                            np.asarray(gla_gate_b[l], f), np.asarray(pool_w[l], f))
    gp = _global_pack(np.asarray(rel_bias, f), np.asarray(final_norm_g, f))
    shared = {"wfm": wfm, "wtm": wtm, "wout": np.ascontiguousarray(np.asarray(w_out, f)[:depth]),
              "lpack": lp, "gpack": gp, "cpack": _consts()}
    return shared


def kernel(x, norm_g, w_in, gla_gate_w2, gla_gate_b, gla_norm_g, rel_bias, pool_w, pool_scale,
           w_out, final_norm_g):
    x = np.asarray(x, np.float32)
    B, T, _ = x.shape
    shared = prep_inputs(x, norm_g, w_in, gla_gate_w2, gla_gate_b, gla_norm_g, rel_bias, pool_w,
                         pool_scale, w_out, final_norm_g)
    nc = build(T=T, depth=DEPTH)
    in_maps = [dict(shared, x=np.ascontiguousarray(x[b])) for b in range(B)]
    res = run_bass_kernel_spmd(nc, in_maps, core_ids=list(range(B)))
    return np.stack([np.asarray(r["y"], np.float32) for r in res.results], axis=0)
```

```python
import math
from contextlib import ExitStack

import numpy as np
import concourse.bass as bass
import concourse.mybir as mybir
from concourse.bass_utils import run_bass_kernel_spmd

F32 = mybir.dt.float32
BF16 = mybir.dt.bfloat16
AF = mybir.ActivationFunctionType
ALU = mybir.AluOpType
AX = mybir.AxisListType

D = 1024
DEPTH = 4
SEQ = 8192
NCORES = 8
EPS = 1e-6
TOPK = 256
N_BISECT = 16
NEG = -1.0e30

O_GQ, O_GK, O_GV, O_GZ, O_GG = 0, 192, 384, 768, 784
O_DQ, O_DK, O_DV, O_DG = 1168, 1552, 1936, 2320
O_QI, O_KI, O_WI, O_PU, O_PG = 2704, 2832, 2864, 2868, 3124

NFM = 13 * 128
NTM = 2308
TM_GROUPS = [(0, 512), (512, 512), (1024, 388), (1412, 512), (1924, 384)]

C_TRI, C_UT, C_MASK4, C_MD, C_MP, C_MD0, C_ID, C_POW2, C_NEGM = 0, 128, 256, 768, 1280, 1792, 2304, 2432, 2464
NCONST = 2592
P_NG, P_GN, P_PS, P_W2, P_PW = 0, 1024, 1408, 1664, 1920
NPL = 2176
G_BD, G_BP, G_C31, G_FN = 0, 768, 1536, 1544
NGP = 2568


def _pairpad(w48x4):
    r = w48x4.shape[0]
    out = np.zeros((r, 256), np.float32)
    for h in range(4):
        out[:, h * 64:h * 64 + 48] = w48x4[:, h * 48:(h + 1) * 48]
    return out


def _t5_bucket(rel):
    rel = np.maximum(rel, 0)
    relf = np.maximum(rel, 1).astype(np.float32)
    large = 16 + (np.log(relf / np.float32(16)) / np.float32(math.log(128 / 16))
                  * np.float32(16)).astype(np.int32)
    large = np.minimum(large, 31)
    return np.where(rel < 16, rel, large)


def _consts():
    c = np.zeros((128, NCONST), np.float32)
    j = np.arange(128)[:, None]
    i = np.arange(128)[None, :]
    same = (j // 64) == (i // 64)
    c[:, C_TRI:C_TRI + 128] = np.where(same & (j <= i), -1.0 / 16.0, 0.0)
    c[:, C_UT:C_UT + 128] = np.where(same & (j > i), -1.0 / 16.0, 0.0)
    m = np.where(same & (j <= i), 1.0, 0.0)
    for h in range(4):
        c[:, C_MASK4 + h * 128:C_MASK4 + (h + 1) * 128] = m
    s = np.arange(128)[:, None]
    t = np.arange(128)[None, :]
    for g, w in enumerate((2, 4, 8, 16)):
        md = np.where((s <= t) & (s > t - w), 1.0 / w, 0.0) - np.where(s == t, 1.0, 0.0)
        mp = np.where((s - 128) > (t - w), 1.0 / w, 0.0)
        cnt = np.minimum(t + 1, w).astype(np.float32)
        md0 = np.where((s <= t) & (s > t - w), 1.0 / cnt, 0.0) - np.where(s == t, 1.0, 0.0)
        c[:, C_MD + g * 128:C_MD + (g + 1) * 128] = md
        c[:, C_MP + g * 128:C_MP + (g + 1) * 128] = mp
        c[:, C_MD0 + g * 128:C_MD0 + (g + 1) * 128] = md0
    c[:, C_ID:C_ID + 128] = np.eye(128, dtype=np.float32)
    c[:, C_POW2:C_POW2 + 32] = (0.5 ** np.arange(1, 33))[None, :]
    c[:, C_NEGM:C_NEGM + 128] = np.where(i > j, NEG, 0.0)
    return c


def _layer_pack(norm_g, gla_norm_g, pool_scale, w2, gate_b, pool_w):
    p = np.zeros((128, NPL), np.float32)
    p[:, P_NG:P_NG + 1024] = norm_g[None, :]
    p[:, P_GN:P_GN + 384] = np.tile(gla_norm_g, 4)[None, :]
    p[:, P_PS:P_PS + 256] = pool_scale[None, :]
    p[0:16, P_W2:P_W2 + 256] = _pairpad(w2)
    p[32, P_W2:P_W2 + 256] = _pairpad(gate_b[None, :])[0]
    for g in range(4):
        p[0:64, P_PW + g * 64:P_PW + (g + 1) * 64] = pool_w[g]
    return p


def _global_pack(rel_bias, final_norm_g):
    gp = np.zeros((128, NGP), np.float32)
    s = np.arange(128)[:, None]
    t = np.arange(128)[None, :]
    bd = _t5_bucket(t - s)
    bp = _t5_bucket(t - s + 128)
    for h in range(6):
        gp[:, G_BD + h * 128:G_BD + (h + 1) * 128] = rel_bias[bd, h]
        gp[:, G_BP + h * 128:G_BP + (h + 1) * 128] = rel_bias[bp, h]
    gp[:, G_C31:G_C31 + 6] = rel_bias[31][None, :]
    gp[:, G_FN:G_FN + 1024] = final_norm_g[None, :]
    return gp


def _w_layouts(w_in_l):
    wfm = np.zeros((D, NFM), np.float32)
    gq = _pairpad(w_in_l[:, O_GQ:O_GQ + 192])
    gk = _pairpad(w_in_l[:, O_GK:O_GK + 192])
    wfm[:, 0:256] = gq
    wfm[:, 256:512] = gk
    wfm[:, 512:528] = w_in_l[:, O_GZ:O_GZ + 16]
    wfm[:, 640:1024] = w_in_l[:, O_DQ:O_DQ + 384]
    wfm[:, 1024:1408] = w_in_l[:, O_DK:O_DK + 384]
    wfm[:, 1408:1536] = w_in_l[:, O_QI:O_QI + 128]
    for r in range(4):
        wfm[:, 1536 + r * 32:1536 + (r + 1) * 32] = w_in_l[:, O_KI:O_KI + 32]
    wtm = np.zeros((D, NTM), np.float32)
    wtm[:, 0:384] = w_in_l[:, O_GG:O_GG + 384]
    wtm[:, 384:768] = w_in_l[:, O_DG:O_DG + 384]
    wtm[:, 768:1024] = w_in_l[:, O_PG:O_PG + 256]
    wtm[:, 1024:1408] = w_in_l[:, O_GV:O_GV + 384]
    wtm[:, 1408:1412] = w_in_l[:, O_WI:O_WI + 4]
    wtm[:, 1412:1668] = gk
    wtm[:, 1668:1924] = w_in_l[:, O_PU:O_PU + 256]
    wtm[:, 1924:2308] = w_in_l[:, O_DV:O_DV + 384]
    return wfm, wtm


class _Stop(Exception):
    pass


class Res:
    __slots__ = ("name", "w", "r", "excl")

    def __init__(self, name, excl=False):
        self.name = name
        self.w = {}
        self.r = {}
        self.excl = excl


class TK:
    def __init__(self, nc, es):
        self.nc = nc
        self.es = es
        self.eng = {"pe": nc.tensor, "dve": nc.vector, "act": nc.scalar, "pool": nc.gpsimd,
                    "sp": nc.sync}
        self.sem = {}
        self.cnt = {}
        self.seen = {e: {} for e in self.eng}
        for e in ("pe", "dve", "act", "pool"):
            self.sem[e] = es.enter_context(nc.semaphore("sem_" + e))
            self.cnt[e] = 0
        self.ninst = 0

    def _wait(self, e, deps):
        for k, c in deps.items():
            if c > 0 and self.seen[e].get(k, 0) < c:
                self.eng[e].wait_ge(self.sem[k], c)
                self.seen[e][k] = c
                self.ninst += 1

    def _deps(self, e, reads, writes):
        deps = {}
        for r in reads:
            for k, c in r.w.items():
                if k == e and e == "pe":
                    continue
                if deps.get(k, 0) < c:
                    deps[k] = c
        for w in writes:
            for k, c in list(w.w.items()) + list(w.r.items()):
                if k == e and e == "pe":
                    continue
                if deps.get(k, 0) < c:
                    deps[k] = c
        return deps

    def op(self, e, fn, R=(), W=(), serial=False):
        W = list(W) + [r for r in R if r.excl]
        R = [r for r in R if not r.excl]
        deps = self._deps(e, R, W)
        if serial and self.cnt[e] > 0:
            deps[e] = self.cnt[e]
        self._wait(e, deps)
        inst = fn(self.eng[e])
        self.cnt[e] += 1
        c = self.cnt[e]
        inst.then_inc(self.sem[e], 1)
        self.ninst += 1
        for r in R:
            r.r[e] = c
        for w in W:
            w.w = {e: c}
            w.r = {}
        return inst

    def pe(self, fn, R=(), W=(), serial=False):
        return self.op("pe", fn, R, W, serial)

    def dve(self, fn, R=(), W=()):
        return self.op("dve", fn, R, W)

    def act(self, fn, R=(), W=()):
        return self.op("act", fn, R, W)

    def pool(self, fn, R=(), W=()):
        return self.op("pool", fn, R, W)

    def stream(self, name):
        if name not in self.sem:
            self.sem[name] = self.es.enter_context(self.nc.semaphore("ds_" + name))
            self.cnt[name] = 0
        return name

    def dma(self, st, out, in_, R=(), W=(), q="sp"):
        deps = self._deps(q, R, W)
        if self.cnt[st] > 0:
            deps[st] = max(deps.get(st, 0), self.cnt[st])
        self._wait(q, deps)
        inst = self.eng[q].dma_start(out=out, in_=in_)
        self.cnt[st] += 16
        c = self.cnt[st]
        inst.then_inc(self.sem[st], 16)
        self.ninst += 1
        for r in R:
            r.r[st] = c
        for w in W:
            w.w = {st: c}
            w.r = {}

    def barrier(self):
        allc = {k: c for k, c in self.cnt.items() if c > 0}
        for e in self.eng:
            self._wait(e, dict(allc))


import os as _os
_DIS = set(_os.environ.get("DISABLE", "").split(","))
GPE = "dve" if "pool" in _DIS else "pool"


def build(T=SEQ, depth=DEPTH, dbg=False, stop_after=None):
    topk = min(TOPK, T // 4)
    QB0 = topk // 128
    NT = T // 128
    NG = T // 512
    nc = bass.Bass("TRN2", target_bir_lowering=False)
    okind = "ExternalOutput" if dbg else "Internal"

    x_d = nc.dram_tensor("x", [T, D], F32, kind="ExternalInput").ap()
    wfm_d = nc.dram_tensor("wfm", [depth, D, NFM], F32, kind="ExternalInput").ap()
    wtm_d = nc.dram_tensor("wtm", [depth, D, NTM], F32, kind="ExternalInput").ap()
    wout_d = nc.dram_tensor("wout", [depth, D, D], F32, kind="ExternalInput").ap()
    lp_d = nc.dram_tensor("lpack", [depth, 128, NPL], F32, kind="ExternalInput").ap()
    gp_d = nc.dram_tensor("gpack", [128, NGP], F32, kind="ExternalInput").ap()
    cst_d = nc.dram_tensor("cpack", [128, NCONST], F32, kind="ExternalInput").ap()
    y_d = nc.dram_tensor("y", [T, D], F32, kind="ExternalOutput").ap()

    xres_d = nc.dram_tensor("xres", [T, D], F32, kind="Internal").ap()
    sqT_d = nc.dram_tensor("s_qT", [128, 3, T], BF16, kind=okind).ap()
    skT_d = nc.dram_tensor("s_kT", [128, 3, T], BF16, kind=okind).ap()
    sqi_d = nc.dram_tensor("s_qi", [2, 64, T], BF16, kind=okind).ap()
    ski_d = nc.dram_tensor("s_ki", [128, T], BF16, kind=okind).ap()
    sv_d = nc.dram_tensor("s_v", [T, 390], BF16, kind=okind).ap()
    sym_d = nc.dram_tensor("s_ymix", [T, D], BF16, kind=okind).ap()
    swi_d = nc.dram_tensor("s_wi", [T, 4], F32, kind=okind).ap()
    smk_d = nc.dram_tensor("s_mask", [NG, NT, 128, 512], BF16, kind=okind).ap()

    with ExitStack() as es:
        tk = TK(nc, es)

        def sb(name, shape, dt):
            return es.enter_context(nc.sbuf_tensor(name, shape, dt))

        banks = [es.enter_context(nc.psum_tensor(f"bank{i}", [128, 512], F32)) for i in range(8)]
        rbank = [Res(f"bank{i}", excl=True) for i in range(8)]

        r_x = [Res(f"x{b}") for b in range(NT)]
        r_sq = [Res(f"sq{g}") for g in range(NG)]
        r_sk = [Res(f"sk{g}") for g in range(NG)]
        r_sqi = [Res(f"sqi{g}") for g in range(NG)]
        r_ski = [Res(f"ski{g}") for g in range(NG)]
        r_sv = [Res(f"sv{b}") for b in range(NT)]
        r_sym = [Res(f"sym{b}") for b in range(NT)]
        r_swi = [Res(f"swi{b}") for b in range(NT)]
        r_smk = [Res(f"smk{b}") for b in range(NT)]

        cst = sb("cst", [128, NCONST], F32)
        r_cst = Res("cst")
        cbf = sb("cbf", [128, 1664], BF16)
        r_cbf = Res("cbf")
        gpk = sb("gpk", [128, NGP], F32)
        r_gpk = Res("gpk")
        epst = sb("epst", [128, 1], F32)
        r_eps = Res("eps")
        st_c = tk.stream("cst")
        st_g = tk.stream("gpk")
        tk.dma(st_c, cst[:], cst_d[:, :], W=[r_cst])
        tk.dma(st_g, gpk[:], gp_d[:, :], W=[r_gpk])
        tk.dve(lambda e: e.tensor_copy(out=cbf[:], in_=cst[:, C_MD:C_MD + 1664]), R=[r_cst], W=[r_cbf])
        tk.dve(lambda e: e.memset(epst[:], EPS), W=[r_eps])
        ids = sb("ids", [128, 128], BF16)
        r_ids = Res("ids")
        tk.dve(lambda e: e.tensor_scalar(out=ids[:], in0=cst[:, C_ID:C_ID + 128], scalar1=30000.0, scalar2=None,
                                         op0=ALU.mult), R=[r_cst], W=[r_ids])
        MDb = cbf[:, 0:512]
        MPb = cbf[:, 512:1024]
        MD0b = cbf[:, 1024:1536]
        IDb = cbf[:, 1536:1664]
        for h in range(6):
            for off in (G_BD, G_BP):
                sl = gpk[:, off + h * 128:off + (h + 1) * 128]
                tk.dve(lambda e, sl=sl, h=h: e.tensor_scalar(
                    out=sl, in0=sl, scalar1=gpk[:, G_C31 + h:G_C31 + h + 1], scalar2=8.0,
                    op0=ALU.subtract, op1=ALU.mult), R=[r_gpk], W=[r_gpk])

        st_misc = [tk.stream(f"misc{i}") for i in range(4)]

        def layer(l):
            x_src = x_d if l == 0 else xres_d
            last = (l == depth - 1)
            def phase_a(pa):
                def sa(name, shape, dt):
                    return pa.enter_context(nc.sbuf_tensor(f"a{l}_{name}", shape, dt))

                wfm = sa("wfm", [128, 8, NFM], BF16)
                wtm = sa("wtm", [128, 8, NTM], BF16)
                r_w = Res("w")
                lpk = sa("lpk", [128, NPL], F32)
                r_lpk = Res("lpk")
                pwb = sa("pwb", [64, 256], BF16)
                r_pwb = Res("pwb")
                tk.dma(st_misc[0], lpk[:], lp_d[l, :, :], W=[r_lpk])
                tk.act(lambda e: e.copy(out=pwb[:], in_=lpk[0:64, P_PW:P_PW + 256]), R=[r_lpk], W=[r_pwb])
                with ExitStack() as ws:
                    wst = [ws.enter_context(nc.sbuf_tensor(f"a{l}_wst{i}", [128, 8, 256], F32)) for i in range(2)]
                    r_wst = [Res("wst0"), Res("wst1")]
                    st_w = st_misc[1:3]
                    ci = 0
                    for (src, dst, ncol) in ((wfm_d, wfm, NFM), (wtm_d, wtm, NTM)):
                        srcv = src[l].rearrange("(k p) c -> p k c", p=128)
                        c0 = 0
                        while c0 < ncol:
                            cw = min(256, ncol - c0)
                            s = ci % 2
                            tk.dma(st_w[s], wst[s][:, :, 0:cw], srcv[:, :, c0:c0 + cw], W=[r_wst[s]])
                            engs = ("act", "dve", "pool")
                            en = engs[ci % 3]
                            if en == "act":
                                tk.act(lambda e, s=s, cw=cw, c0=c0, dst=dst: e.copy(out=dst[:, :, c0:c0 + cw], in_=wst[s][:, :, 0:cw]),
                                       R=[r_wst[s]], W=[r_w])
                            else:
                                tk.op(en, lambda e, s=s, cw=cw, c0=c0, dst=dst: e.tensor_copy(out=dst[:, :, c0:c0 + cw], in_=wst[s][:, :, 0:cw]),
                                      R=[r_wst[s]], W=[r_w])
                            c0 += cw
                            ci += 1
                    tk.barrier()
                if stop_after == "weights":
                    raise _Stop()
                r_w = Res("w2")

                normg = lpk[:, P_NG:P_NG + 1024]
                gnorm4 = lpk[:, P_GN:P_GN + 384]
                pscale = lpk[:, P_PS:P_PS + 256]
                w2aug = lpk[0:33, P_W2:P_W2 + 256]

                NXS = 2
                xs = [sa(f"x{i}", [128, D], F32) for i in range(NXS)]
                r_xs = [Res(f"xs{i}") for i in range(NXS)]
                st_x = [tk.stream(f"a_x{i}") for i in range(NXS)]
                junk = sa("junk", [128, D], BF16)
                ssq = sa("ssq", [128, 8], F32)
                r_ssq = Res("ssq")
                hb = [sa(f"h{i}", [128, D], BF16) for i in range(2)]
                r_hb = [Res("h0"), Res("h1")]
                hT = [sa(f"hT{i}", [128, 8, 512], BF16) for i in range(2)]
                r_hT = [Res("hT0"), Res("hT1")]
                fmg = [sa(f"fmg{i}", [128, 4, 512], BF16) for i in range(2)]
                r_fmg = [Res("fmg0"), Res("fmg1")]
                gzs = [sa(f"gz{i}", [64, 512], F32) for i in range(2)]
                r_gzs = [Res("gz0"), Res("gz1")]
                fmq = sa("fmq", [128, 3, 512], BF16)
                fmk = sa("fmk", [128, 3, 512], BF16)
                fmqi = sa("fmqi", [128, 512], BF16)
                fmki = sa("fmki", [128, 512], BF16)
                r_fmq, r_fmk, r_fmqi, r_fmki = Res("fmq"), Res("fmk"), Res("fmqi"), Res("fmki")
                st_fm = [tk.stream(f"a_fm{i}") for i in range(4)]
                sg = sa("sg", [128, 1024], F32)
                r_sg = Res("sg")
                gg = sa("gg", [128, 384], F32)
                r_gg = Res("gg")
                psg = sa("psg", [128, 256], F32)
                r_psg = Res("psg")
                vgl = sa("vgl", [128, 384], BF16)
                r_vgl = Res("vgl")
                ktm = sa("ktm", [128, 256], F32)
                r_ktm = Res("ktm")
                pus = [sa(f"pu{i}", [128, 256], BF16) for i in range(2)]
                r_pus = [Res("pu0"), Res("pu1")]
                vaug = [sa(f"vaug{i}", [128, 6, 65], BF16) for i in range(2)]
                r_vaug = [Res("va0"), Res("va1")]
                st_va = [tk.stream(f"a_va{i}") for i in range(2)]
                ymx = [sa(f"ymx{i}", [128, 1024], BF16) for i in range(2)]
                r_ymx = [Res("ym0"), Res("ym1")]
                st_ym = [tk.stream(f"a_ym{i}") for i in range(2)]
                wis = [sa(f"wis{i}", [128, 4], F32) for i in range(2)]
                r_wis = [Res("wi0"), Res("wi1")]
                st_wi = [tk.stream(f"a_wi{i}") for i in range(2)]
                lt = sa("lt", [128, 256], F32)
                r_lt = Res("lt")
                ex = sa("ex", [128, 256], F32)
                r_ex = Res("ex")
                Eq = sa("Eq", [128, 256], F32)
                Ek = sa("Ek", [128, 256], F32)
                Ekd = sa("Ekd", [128, 256], F32)
                r_Eq, r_Ek, r_Ekd = Res("Eq"), Res("Ek"), Res("Ekd")
                qeT = sa("qeT", [128, 2, 128], BF16)
                keT = sa("keT", [128, 2, 128], BF16)
                kd = sa("kd", [128, 256], BF16)
                r_qeT, r_keT, r_kd = Res("qeT"), Res("keT"), Res("kd")
                ATs = sa("ATs", [128, 512], BF16)
                r_ATs = Res("ATs")
                S32 = sa("S32", [128, 2, 96], F32)
                S32m = sa("S32m", [128, 2, 96], F32)
                Sba = sa("Sba", [128, 2, 96], BF16)
                Sbb = sa("Sbb", [128, 2, 96], BF16)
                r_S32, r_S32m, r_Sba, r_Sbb = Res("S32"), Res("S32m"), Res("Sba"), Res("Sbb")
                rt4 = sa("rt4", [128, 8], F32)
                r_rt4 = Res("rt4")
                rstd4 = sa("rstd4", [128, 8], F32)
                r_rstd4 = Res("rstd4")
                pTs = sa("pTs", [64, 512], BF16)
                r_pTs = Res("pTs")

                tk.dve(lambda e: e.memset(S32[:], 0.0), W=[r_S32])
                tk.dve(lambda e: e.memset(Sba[:], 0.0), W=[r_Sba])
                tk.dve(lambda e: e.memset(S32m[:], 0.0), W=[r_S32m])
                tk.dve(lambda e: e.memset(Sbb[:], 0.0), W=[r_Sbb])
                for i in range(2):
                    tk.dve(lambda e, i=i: e.memset(gzs[i][:], 0.0), W=[r_gzs[i]])
                    tk.dve(lambda e, i=i: e.memset(gzs[i][32:33, :], 1.0), W=[r_gzs[i]])
                    tk.dve(lambda e, i=i: e.memset(vaug[i][:], 1.0), W=[r_vaug[i]])

                B_TR, B_FM, B_TM, B_G0, B_G1, B_G2 = 0, (1, 2), (3, 4), 5, 6, 7
                tr_bf = banks[B_TR].bitcast(BF16)

                def load_x(blk):
                    s = blk % NXS
                    tk.dma(st_x[s], xs[s][:], x_src[blk * 128:(blk + 1) * 128, :], R=[r_x[blk]], W=[r_xs[s]])

                for b0 in range(min(NXS, NT)):
                    load_x(b0)

                evac_rr = [0]

                def evac(out, in_, R, W):
                    evac_rr[0] += 1
                    if evac_rr[0] % 2:
                        tk.act(lambda e: e.copy(out=out, in_=in_), R=R, W=W)
                    else:
                        tk.dve(lambda e: e.tensor_copy(out=out, in_=in_), R=R, W=W)

                for tg in range(NG):
                    hs = tg % 2
                    for j in range(4):
                        blk = tg * 4 + j
                        s = blk % NXS
                        hh = blk % 2
                        tk.act(lambda e, s=s: e.activation(out=junk[:], in_=xs[s][:], func=AF.Square,
                                                            accum_out=ssq[:, 0:1]),
                               R=[r_xs[s]], W=[r_ssq])
                        tk.act(lambda e: e.activation(out=ssq[:, 1:2], in_=ssq[:, 0:1], func=AF.Sqrt,
                                                      bias=epst[:, 0:1], scale=1.0 / D),
                               R=[r_ssq, r_eps], W=[r_ssq])
                        tk.dve(lambda e: e.reciprocal(out=ssq[:, 2:3], in_=ssq[:, 1:2]), R=[r_ssq], W=[r_ssq])
                        tk.dve(lambda e, s=s, hh=hh: e.scalar_tensor_tensor(
                            out=hb[hh][:], in0=xs[s][:], scalar=ssq[:, 2:3], in1=normg,
                            op0=ALU.mult, op1=ALU.mult), R=[r_xs[s], r_ssq, r_lpk], W=[r_hb[hh]])
                        if blk + NXS < NT:
                            load_x(blk + NXS)
                        for k in range(8):
                            tk.pe(lambda e, k=k, hh=hh: e.transpose(
                                out=tr_bf[:, k * 128:(k + 1) * 128], in_=hb[hh][:, k * 128:(k + 1) * 128],
                                identity=IDb), R=[r_hb[hh], r_cbf], W=[rbank[B_TR]])
                        evac(hT[hs][:, :, j * 128:(j + 1) * 128],
                             tr_bf[:, :].rearrange("p (k c) -> p k c", k=8),
                             R=[rbank[B_TR]], W=[r_hT[hs]])
                    if stop_after == "norm":
                        raise _Stop()
                    for cb in range(13):
                        bk = B_FM[cb % 2]
                        M = 64 if cb == 4 else 128
                        for k in range(8):
                            tk.pe(lambda e, k=k, cb=cb, bk=bk, M=M: e.matmul(
                                out=banks[bk][0:M, :], lhsT=wfm[:, k, cb * 128:cb * 128 + M],
                                rhs=hT[hs][:, k, :], start=(k == 0), stop=(k == 7)),
                                R=[r_hT[hs], r_w], W=[rbank[bk]])
                        if cb < 4:
                            evac(fmg[hs][:, cb, :], banks[bk][:, :], R=[rbank[bk]], W=[r_fmg[hs]])
                        elif cb == 4:
                            evac(gzs[hs][0:32, :], banks[bk][0:32, :], R=[rbank[bk]], W=[r_gzs[hs]])
                        elif cb < 8:
                            evac(fmq[:, cb - 5, :], banks[bk][:, :], R=[rbank[bk]], W=[r_fmq])
                        elif cb < 11:
                            evac(fmk[:, cb - 8, :], banks[bk][:, :], R=[rbank[bk]], W=[r_fmk])
                        elif cb == 11:
                            evac(fmqi[:], banks[bk][:, :], R=[rbank[bk]], W=[r_fmqi])
                        else:
                            evac(fmki[:], banks[bk][:, :], R=[rbank[bk]], W=[r_fmki])
                    tsl = slice(tg * 512, (tg + 1) * 512)
                    tk.dma(st_fm[0], sqT_d[:, :, tsl], fmq[:], R=[r_fmq], W=[r_sq[tg]])
                    tk.dma(st_fm[1], skT_d[:, :, tsl], fmk[:], R=[r_fmk], W=[r_sk[tg]])
                    tk.dma(st_fm[2], sqi_d[0, :, tsl], fmqi[0:64, :], R=[r_fmqi], W=[r_sqi[tg]])
                    tk.dma(st_fm[2], sqi_d[1, :, tsl], fmqi[64:128, :], R=[r_fmqi], W=[r_sqi[tg]])
                    tk.dma(st_fm[3], ski_d[:, tsl], fmki[:], R=[r_fmki], W=[r_ski[tg]])

                    if stop_after == "fm":
                        raise _Stop()
                    for j in range(4):
                        blk = tg * 4 + j
                        s2 = blk % 2
                        jsl = slice(j * 128, (j + 1) * 128)

                        def tm_mm(gi, bk):
                            c0, cw = TM_GROUPS[gi]
                            for k in range(8):
                                tk.pe(lambda e, k=k: e.matmul(
                                    out=banks[bk][:, 0:cw], lhsT=hT[hs][:, k, jsl],
                                    rhs=wtm[:, k, c0:c0 + cw], start=(k == 0), stop=(k == 7)),
                                    R=[r_hT[hs], r_w], W=[rbank[bk]])

                        for gi in (0, 1):
                            bk = B_TM[gi]
                            tm_mm(gi, bk)
                            tk.act(lambda e, gi=gi, bk=bk: e.activation(
                                out=sg[:, gi * 512:(gi + 1) * 512], in_=banks[bk][:, :], func=AF.Silu),
                                R=[rbank[bk]], W=[r_sg])
                        if stop_after == "tm0":
                            raise _Stop()
                        bk = B_TM[0]
                        tm_mm(2, bk)
                        tk.dve(lambda e, bk=bk: e.tensor_copy(out=vgl[:], in_=banks[bk][:, 0:384]),
                               R=[rbank[bk]], W=[r_vgl])
                        tk.act(lambda e, bk=bk: e.copy(out=wis[s2][:], in_=banks[bk][:, 384:388]),
                               R=[rbank[bk]], W=[r_wis[s2]])
                        if "wi" not in _DIS:
                            tk.dma(st_wi[s2], swi_d[blk * 128:(blk + 1) * 128, :], wis[s2][:], R=[r_wis[s2]], W=[r_swi[blk]])
                        if stop_after == "tm1":
                            raise _Stop()
                        bk = B_TM[1]
                        if "mm3" not in _DIS:
                            tm_mm(3, bk)
                        if "ktm" not in _DIS:
                            tk.act(lambda e, bk=bk: e.copy(out=ktm[:], in_=banks[bk][:, 0:256]),
                                   R=[rbank[bk]], W=[r_ktm])
                        if "pus" not in _DIS:
                            tk.dve(lambda e, bk=bk: e.tensor_copy(out=pus[s2][:], in_=banks[bk][:, 256:512]),
                                   R=[rbank[bk]], W=[r_pus[s2]])
                        if stop_after == "tm2":
                            raise _Stop()
                        bk = B_TM[0]
                        tm_mm(4, bk)
                        tk.act(lambda e, bk=bk: e.copy(
                            out=vaug[s2][:, :, 0:64],
                            in_=banks[bk][:, 0:384].rearrange("p (h d) -> p h d", h=6)),
                            R=[rbank[bk]], W=[r_vaug[s2]])
                        if "va" not in _DIS:
                            tk.dma(st_va[s2], sv_d[blk * 128:(blk + 1) * 128, :],
                                   vaug[s2][:].rearrange("p h d -> p (h d)"), R=[r_vaug[s2]], W=[r_sv[blk]])
                        tk.op(GPE, lambda e: e.tensor_copy(out=ymx[s2][:, 384:768], in_=sg[:, 384:768]),
                                R=[r_sg], W=[r_ymx[s2]])
                        tk.op(GPE, lambda e: e.tensor_tensor(out=gg[:], in0=sg[:, 0:384], in1=gnorm4, op=ALU.mult),
                                R=[r_sg, r_lpk], W=[r_gg])
                        tk.op(GPE, lambda e: e.tensor_tensor(out=psg[:], in0=sg[:, 768:1024], in1=pscale, op=ALU.mult),
                                R=[r_sg, r_lpk], W=[r_psg])

                        if stop_after == "tm":
                            raise _Stop()
                        tk.pe(lambda e: e.matmul(out=banks[B_G0][:, 0:256], lhsT=gzs[hs][0:33, jsl],
                                                 rhs=w2aug, start=True, stop=True),
                              R=[r_gzs[hs], r_lpk], W=[rbank[B_G0]])
                        tk.act(lambda e: e.activation(out=ex[:], in_=banks[B_G0][:, 0:256], func=AF.Exp, scale=-1.0),
                               R=[rbank[B_G0]], W=[r_ex])
                        tk.act(lambda e: e.activation(out=lt[:], in_=ex[:], func=AF.Ln, bias=1.0),
                               R=[r_ex], W=[r_lt])
                        for p in range(2):
                            tk.pe(lambda e, p=p: e.matmul(
                                out=banks[B_G0][:, p * 128:(p + 1) * 128], lhsT=lt[:, p * 128:(p + 1) * 128],
                                rhs=cst[:, C_TRI:C_TRI + 128], start=True, stop=True),
                                R=[r_lt, r_cst], W=[rbank[B_G0]])
                        tk.pe(lambda e: e.matmul(out=banks[B_G0][:, 256:512], lhsT=cst[:, C_UT:C_UT + 128],
                                                 rhs=lt[:], start=True, stop=True),
                              R=[r_lt, r_cst], W=[rbank[B_G0]])
                        tk.act(lambda e: e.activation(out=Eq[:], in_=banks[B_G0][:, 0:256], func=AF.Exp),
                               R=[rbank[B_G0]], W=[r_Eq])
                        tk.act(lambda e: e.activation(out=Ek[:], in_=banks[B_G0][:, 0:256], func=AF.Exp, scale=-1.0),
                               R=[rbank[B_G0]], W=[r_Ek])
                        tk.act(lambda e: e.activation(out=Ekd[:], in_=banks[B_G0][:, 256:512], func=AF.Exp),
                               R=[rbank[B_G0]], W=[r_Ekd])
                        tk.dve(lambda e: e.scalar_tensor_tensor(
                            out=qeT[:], in0=fmg[hs][:, 0:2, jsl], scalar=48.0 ** -0.5,
                            in1=Eq[:].rearrange("p (a t) -> p a t", a=2), op0=ALU.mult, op1=ALU.mult),
                            R=[r_fmg[hs], r_Eq], W=[r_qeT])
                        tk.dve(lambda e: e.tensor_tensor(
                            out=keT[:], in0=fmg[hs][:, 2:4, jsl],
                            in1=Ek[:].rearrange("p (a t) -> p a t", a=2), op=ALU.mult),
                            R=[r_fmg[hs], r_Ek], W=[r_keT])
                        tk.dve(lambda e: e.tensor_tensor(out=kd[:], in0=ktm[:], in1=Ekd[:], op=ALU.mult),
                               R=[r_ktm, r_Ekd], W=[r_kd])

                        def u_mm(c):
                            for h in range(4):
                                p, base = h // 2, 64 * (h % 2)
                                tk.pe(lambda e, h=h, p=p, base=base: e.matmul(
                                    out=banks[B_G0][base:base + 64, c * 192 + p * 96:c * 192 + (p + 1) * 96],
                                    lhsT=kd[c * 64:(c + 1) * 64, h * 64:(h + 1) * 64],
                                    rhs=vgl[c * 64:(c + 1) * 64, h * 96:(h + 1) * 96], start=True, stop=True),
                                    R=[r_kd, r_vgl], W=[rbank[B_G0]])

                        u_mm(0)
                        for p in range(2):
                            tk.dve(lambda e, p=p: e.scalar_tensor_tensor(
                                out=S32m[:, p, :], in0=S32[:, p, :], scalar=Eq[:, p * 128 + 63:p * 128 + 64],
                                in1=banks[B_G0][:, p * 96:(p + 1) * 96], op0=ALU.mult, op1=ALU.add),
                                R=[r_S32, r_Eq, rbank[B_G0]], W=[r_S32m])
                        tk.op(GPE, lambda e: e.tensor_copy(out=Sbb[:], in_=S32m[:]), R=[r_S32m], W=[r_Sbb])
                        for h in range(4):
                            p, base = h // 2, 64 * (h % 2)
                            tk.pe(lambda e, h=h, p=p, base=base: e.matmul(
                                out=banks[B_G1][:, h * 128:(h + 1) * 128], lhsT=keT[base:base + 48, p, :],
                                rhs=qeT[base:base + 48, p, :], start=True, stop=True),
                                R=[r_keT, r_qeT], W=[rbank[B_G1]], serial=(h > 0))
                        tk.dve(lambda e: e.tensor_tensor(out=ATs[:], in0=banks[B_G1][:, :],
                                                         in1=cst[:, C_MASK4:C_MASK4 + 512], op=ALU.mult),
                               R=[rbank[B_G1], r_cst], W=[r_ATs])
                        for h in range(4):
                            p, base = h // 2, 64 * (h % 2)
                            osl = slice(h * 96, (h + 1) * 96)
                            tk.pe(lambda e, h=h, osl=osl: e.matmul(
                                out=banks[B_G2][:, osl], lhsT=ATs[:, h * 128:(h + 1) * 128],
                                rhs=vgl[:, osl], start=True, stop=False),
                                R=[r_ATs, r_vgl], W=[rbank[B_G2]])
                            tk.pe(lambda e, p=p, base=base, osl=osl: e.matmul(
                                out=banks[B_G2][0:64, osl], lhsT=qeT[base:base + 48, p, 0:64],
                                rhs=Sba[base:base + 48, p, :], start=False, stop=True),
                                R=[r_qeT, r_Sba], W=[rbank[B_G2]])
                            tk.pe(lambda e, p=p, base=base, osl=osl: e.matmul(
                                out=banks[B_G2][64:128, osl], lhsT=qeT[base:base + 48, p, 64:128],
                                rhs=Sbb[base:base + 48, p, :], start=False, stop=True),
                                R=[r_qeT, r_Sbb], W=[rbank[B_G2]])
                        u_mm(1)
                        for p in range(2):
                            tk.dve(lambda e, p=p: e.scalar_tensor_tensor(
                                out=S32[:, p, :], in0=S32m[:, p, :], scalar=Eq[:, p * 128 + 127:p * 128 + 128],
                                in1=banks[B_G0][:, 192 + p * 96:192 + (p + 1) * 96], op0=ALU.mult, op1=ALU.add),
                                R=[r_S32m, r_Eq, rbank[B_G0]], W=[r_S32])
                        tk.op(GPE, lambda e: e.tensor_copy(out=Sba[:], in_=S32[:]), R=[r_S32], W=[r_Sba])
                        for h in range(4):
                            tk.act(lambda e, h=h: e.activation(
                                out=junk[:, 0:96], in_=banks[B_G2][:, h * 96:(h + 1) * 96], func=AF.Square,
                                accum_out=rt4[:, h:h + 1]), R=[rbank[B_G2]], W=[r_rt4])
                        tk.act(lambda e: e.activation(out=rt4[:, 4:8], in_=rt4[:, 0:4], func=AF.Sqrt,
                                                      bias=epst[:, 0:1], scale=1.0 / 96.0),
                               R=[r_rt4, r_eps], W=[r_rt4])
                        tk.dve(lambda e: e.reciprocal(out=rstd4[:, 0:4], in_=rt4[:, 4:8]), R=[r_rt4], W=[r_rstd4])
                        for h in range(4):
                            tk.dve(lambda e, h=h: e.scalar_tensor_tensor(
                                out=ymx[s2][:, h * 96:(h + 1) * 96], in0=banks[B_G2][:, h * 96:(h + 1) * 96],
                                scalar=rstd4[:, h:h + 1], in1=gg[:, h * 96:(h + 1) * 96],
                                op0=ALU.mult, op1=ALU.mult),
                                R=[rbank[B_G2], r_rstd4, r_gg], W=[r_ymx[s2]])

                        if stop_after == "gla":
                            raise _Stop()
                        md = MD0b if blk == 0 else MDb
                        for g in range(4):
                            tk.pe(lambda e, g=g: e.matmul(
                                out=banks[B_G1][0:64, g * 128:(g + 1) * 128], lhsT=pus[s2][:, g * 64:(g + 1) * 64],
                                rhs=md[:, g * 128:(g + 1) * 128], start=True, stop=(blk == 0)),
                                R=[r_pus[s2], r_cbf], W=[rbank[B_G1]])
                            if blk > 0:
                                tk.pe(lambda e, g=g: e.matmul(
                                    out=banks[B_G1][0:64, g * 128:(g + 1) * 128],
                                    lhsT=pus[1 - s2][:, g * 64:(g + 1) * 64],
                                    rhs=MPb[:, g * 128:(g + 1) * 128], start=False, stop=True),
                                    R=[r_pus[1 - s2], r_cbf], W=[rbank[B_G1]])
                        tk.act(lambda e: e.copy(out=pTs[:], in_=banks[B_G1][0:64, :]), R=[rbank[B_G1]], W=[r_pTs])
                        for g in range(4):
                            tk.pe(lambda e, g=g: e.matmul(
                                out=banks[B_G1][:, g * 64:(g + 1) * 64], lhsT=pTs[0:64, g * 128:(g + 1) * 128],
                                rhs=pwb[0:64, g * 64:(g + 1) * 64], start=True, stop=True),
                                R=[r_pTs, r_pwb], W=[rbank[B_G1]])
                        tk.dve(lambda e: e.tensor_tensor(out=ymx[s2][:, 768:1024], in0=banks[B_G1][:, 0:256],
                                                         in1=psg[:], op=ALU.mult),
                               R=[rbank[B_G1], r_psg], W=[r_ymx[s2]])
                        tk.dma(st_ym[s2], sym_d[blk * 128:(blk + 1) * 128, :], ymx[s2][:], R=[r_ymx[s2]], W=[r_sym[blk]])
                tk.barrier()

            with ExitStack() as pa:
                try:
                    phase_a(pa)
                except _Stop:
                    stopped[0] = True
            if stopped[0]:
                return
            def phase_b1(pb):
                def sbt(name, shape, dt):
                    return pb.enter_context(nc.sbuf_tensor(f"b{l}_{name}", shape, dt))

                ki4 = sbt("ki4", [128, T], BF16)
                r_ki4 = Res("ki4")
                scores = [sbt(f"scores{i}", [128, T], F32) for i in range(2)]
                r_sc = [Res("scores0"), Res("scores1")]
                junkb = sbt("junkb", [128, T], BF16)
                maskb = sbt("maskb", [128, T], BF16)
                r_mask = Res("maskb")
                mst = [sbt(f"mst{i}", [128, NT, 128], BF16) for i in range(2)]
                r_mst = [Res("mst0"), Res("mst1")]
                st_mst = [tk.stream(f"b_mst{i}") for i in range(2)]
                qis = [sbt(f"qis{i}", [64, 2, 128], BF16) for i in range(2)]
                r_qis = [Res("qis0"), Res("qis1")]
                st_qi = [tk.stream(f"b_qi{i}") for i in range(2)]
                wib = [sbt(f"wib{i}", [128, 4], F32) for i in range(2)]
                r_wib = [Res("wib0"), Res("wib1")]
                st_wib = [tk.stream(f"b_wib{i}") for i in range(2)]
                rh = [sbt(f"rh{i}", [128, 4, 512], BF16) for i in range(2)]
                r_rh = [Res("rh0"), Res("rh1")]
                dgw = [sbt(f"dgw{i}", [128, 4, 128], BF16) for i in range(2)]
                r_dgw = [Res("dgw0"), Res("dgw1")]
                bis = sbt("bis", [128, 8], F32)
                r_bis = Res("bis")
                bw = sbt("bw", [128, 32], F32)
                r_bw = Res("bw")
                st_k = tk.stream(f"b_ki")
                nch = max(1, T // 2048)
                cwk = T // nch
                for c in range(nch):
                    tk.dma(st_k, ki4[:, c * cwk:(c + 1) * cwk], ski_d[:, c * cwk:(c + 1) * cwk],
                           R=r_ski, W=[r_ki4])
                trb = [banks[6].bitcast(BF16), banks[7].bitcast(BF16)]
                rr = [0, 0]
                r_mid, r_cd, r_ca, r_lo, r_g, r_M = (Res("mid"), Res("cd"), Res("ca"), Res("lo"), Res("g"), Res("M"))
                r_thr2 = Res("thr2")
                ACT_SHARE = 0.5

                def gen_scores(qb):
                    s = qb % 2
                    S = (qb + 1) * 128
                    g = qb // 4
                    qsl = slice(qb * 128, (qb + 1) * 128)
                    tk.dma(st_qi[s], qis[s][:], sqi_d[:, :, qsl].rearrange("a p t -> p a t"), R=[r_sqi[g]], W=[r_qis[s]])
                    tk.dma(st_wib[s], wib[s][:], swi_d[qsl, :], R=[r_swi[qb]], W=[r_wib[s]])
                    for h in range(4):
                        tk.op(GPE, lambda e, h=h: e.tensor_scalar(
                            out=dgw[s][:, h, :], in0=cst[:, C_ID:C_ID + 128], scalar1=wib[s][:, h:h + 1],
                            scalar2=None, op0=ALU.mult), R=[r_cst, r_wib[s]], W=[r_dgw[s]])
                    nkt = (S + 511) // 512
                    gt0 = rr[0]
                    rr[0] += nkt

                    def kw_(kt):
                        return min(512, S - kt * 512)

                    def score_mm(kt):
                        for h in range(4):
                            tk.pe(lambda e, h=h: e.matmul(
                                out=banks[h][:, 0:kw_(kt)], lhsT=qis[s][32 * (h % 2):32 * (h % 2) + 32, h // 2, :],
                                rhs=ki4[32 * (h % 2):32 * (h % 2) + 32, kt * 512:kt * 512 + kw_(kt)],
                                start=True, stop=True), R=[r_qis[s], r_ki4], W=[rbank[h]])

                    def relu(kt):
                        r = (gt0 + kt) % 2
                        for h in range(4):
                            tk.act(lambda e, h=h: e.activation(
                                out=rh[r][:, h, 0:kw_(kt)], in_=banks[h][:, 0:kw_(kt)], func=AF.Relu,
                                scale=(32.0 ** -0.5) * 0.5), R=[rbank[h]], W=[r_rh[r]])

                    def diag_mm(kt):
                        r = (gt0 + kt) % 2
                        bk = 4 + r
                        for h in range(4):
                            tk.pe(lambda e, h=h: e.matmul(
                                out=banks[bk][:, 0:kw_(kt)], lhsT=dgw[s][:, h, :], rhs=rh[r][:, h, 0:kw_(kt)],
                                start=(h == 0), stop=(h == 3)), R=[r_dgw[s], r_rh[r]], W=[rbank[bk]])

                    def copy(kt):
                        bk = 4 + (gt0 + kt) % 2
                        tk.act(lambda e: e.copy(out=scores[s][:, kt * 512:kt * 512 + kw_(kt)],
                                                in_=banks[bk][:, 0:kw_(kt)]), R=[rbank[bk]], W=[r_sc[s]])

                    for t in range(nkt + 2):
                        if 0 <= t - 1 < nkt:
                            relu(t - 1)
                        if t < nkt:
                            score_mm(t)
                        if 0 <= t - 1 < nkt:
                            diag_mm(t - 1)
                        if 0 <= t - 2 < nkt:
                            copy(t - 2)
                        yield

                def select(qb, filler):
                    s = qb % 2
                    S = (qb + 1) * 128
                    g, j = qb // 4, qb % 4
                    qsl = slice(qb * 128, (qb + 1) * 128)
                    sc = scores[s]
                    rs = r_sc[s]
                    if qb >= QB0:
                        tk.dve(lambda e: e.tensor_reduce(out=bis[:, 0:1], in_=sc[:, 0:S], axis=AX.X, op=ALU.max,
                                                         apply_absolute_value=True), R=[rs], W=[r_M])
                        tk.dve(lambda e: e.tensor_scalar(out=bw[:, 0:N_BISECT], in0=cst[:, C_POW2:C_POW2 + N_BISECT],
                                                         scalar1=bis[:, 0:1], scalar2=2.0001, op0=ALU.mult, op1=ALU.mult),
                               R=[r_cst, r_M], W=[r_bw])
                        tk.dve(lambda e: e.tensor_scalar(out=bis[:, 1:2], in0=bis[:, 0:1], scalar1=-1.0, scalar2=None,
                                                         op0=ALU.mult), R=[r_M], W=[r_lo])
                    else:
                        tk.dve(lambda e: e.memset(bis[:, 1:2], -1.0e29), W=[r_lo])
                    tk.dve(lambda e: e.tensor_tensor(out=sc[:, qsl], in0=sc[:, qsl],
                                                     in1=cst[:, C_NEGM:C_NEGM + 128], op=ALU.add),
                           R=[rs, r_cst], W=[rs])
                    if qb >= QB0:
                        if qb + 1 < NT:
                            nkt_n = ((qb + 2) * 128 + 511) // 512
                            fsteps = [(3.0 if 1 <= t <= nkt_n else 0.0) + (0.6 if 2 <= t <= nkt_n + 1 else 0.0)
                                      for t in range(nkt_n + 2)]
                        else:
                            fsteps = []

                        def split(k):
                            fk = fsteps[k - 1] if 1 <= k <= len(fsteps) else 0.0
                            a_k = (S * 1.045e-3 + 0.25 - fk) / (S * 1.705e-3)
                            a_k = min(0.72, max(0.08, a_k))
                            s1 = min(S, max(128, int(round(S * (1.0 - a_k) / 128.0)) * 128))
                            return s1, S - s1, float(topk) - 0.5 - (S - s1) / 2.0
                        tk.dve(lambda e: e.tensor_tensor(out=bis[:, 2:3], in0=bis[:, 1:2], in1=bw[:, 0:1], op=ALU.add),
                               R=[r_lo, r_bw], W=[r_mid])
                        for k in range(N_BISECT):
                            lastk = (k == N_BISECT - 1)
                            S1, nA, thr = split(k)
                            if nA > 0:
                                tk.act(lambda e: e.activation(out=junkb[:, S1:S], in_=sc[:, S1:S], func=AF.Sign,
                                                              bias=bis[:, 2:3], scale=-1.0, accum_out=bis[:, 5:6]),
                                       R=[rs, r_mid], W=[r_ca])
                            tk.dve(lambda e: e.tensor_scalar(out=junkb[:, 0:S1], in0=sc[:, 0:S1], scalar1=bis[:, 2:3],
                                                             scalar2=None, op0=ALU.is_ge, op1=ALU.add,
                                                             accum_out=bis[:, 3:4]), R=[rs, r_mid], W=[r_cd])
                            if nA > 0:
                                tk.act(lambda e: e.activation(out=bis[:, 6:7], in_=bis[:, 5:6], func=AF.Identity,
                                                              bias=thr, scale=0.5), R=[r_ca], W=[r_thr2])
                                tk.dve(lambda e, k=k: e.tensor_scalar(out=bis[:, 4:5], in0=bis[:, 3:4], scalar1=bis[:, 6:7],
                                                                      scalar2=bw[:, k:k + 1], op0=ALU.is_ge, op1=ALU.mult),
                                       R=[r_cd, r_bw, r_thr2], W=[r_g])
                            else:
                                tk.dve(lambda e, k=k: e.tensor_scalar(out=bis[:, 4:5], in0=bis[:, 3:4], scalar1=thr,
                                                                      scalar2=bw[:, k:k + 1], op0=ALU.is_ge, op1=ALU.mult),
                                       R=[r_cd, r_bw], W=[r_g])
                            if not lastk:
                                tk.dve(lambda e, k=k: e.scalar_tensor_tensor(
                                    out=bis[:, 2:3], in0=bis[:, 2:3], scalar=bw[:, k + 1:k + 2], in1=bis[:, 4:5],
                                    op0=ALU.subtract, op1=ALU.add), R=[r_mid, r_bw, r_g], W=[r_mid])
                            else:
                                tk.dve(lambda e, k=k: e.scalar_tensor_tensor(
                                    out=bis[:, 1:2], in0=bis[:, 2:3], scalar=bw[:, k:k + 1], in1=bis[:, 4:5],
                                    op0=ALU.subtract, op1=ALU.add), R=[r_mid, r_bw, r_g], W=[r_lo])
                            if filler is not None:
                                next(filler, None)
                    if filler is not None:
                        for _ in filler:
                            pass
                    tk.dve(lambda e: e.tensor_scalar(out=maskb[:, 0:S], in0=sc[:, 0:S], scalar1=bis[:, 1:2],
                                                     scalar2=None, op0=ALU.is_ge), R=[rs, r_lo], W=[r_mask])
                    kb0 = 0
                    while kb0 <= qb:
                        n = min(8, qb + 1 - kb0)
                        t = rr[1] % 2
                        rr[1] += 1
                        for i in range(n):
                            tk.pe(lambda e, i=i: e.transpose(out=trb[t][:, i * 128:(i + 1) * 128],
                                                             in_=maskb[:, (kb0 + i) * 128:(kb0 + i + 1) * 128],
                                                             identity=IDb), R=[r_mask, r_cbf], W=[rbank[6 + t]])
                        tk.act(lambda e: e.copy(out=mst[s][:, kb0:kb0 + n, :],
                                                in_=trb[t][:, 0:n * 128].rearrange("p (k c) -> p k c", k=n)),
                               R=[rbank[6 + t]], W=[r_mst[s]])
                        kb0 += n
                    nkbg = 4 * g + 4
                    if qb + 1 < nkbg:
                        tk.op(GPE, lambda e: e.memset(mst[s][:, qb + 1:nkbg, :], 0.0), W=[r_mst[s]])
                    for k0 in range(0, nkbg, 8):
                        k1 = min(nkbg, k0 + 8)
                        tk.dma(st_mst[s], smk_d[g, k0:k1, :, j * 128:(j + 1) * 128].rearrange("k p q -> p k q"),
                               mst[s][:, k0:k1, :], R=[r_mst[s]], W=[r_smk[qb]])

                for _ in gen_scores(0):
                    pass
                for qb in range(NT):
                    select(qb, gen_scores(qb + 1) if qb + 1 < NT else None)
                tk.barrier()

            def phase_b2(pb):
                def sbt(name, shape, dt):
                    return pb.enter_context(nc.sbuf_tensor(f"c{l}_{name}", shape, dt))

                kT = sbt("kT", [128, 3, T], BF16)
                r_kT = Res("kT")
                vA = sbt("vA", [128, NT, 390], BF16)
                r_vA = Res("vA")
                wout = sbt("wout", [128, 8, D], BF16)
                r_wo = Res("wout")
                st_kv = [tk.stream(f"c_kv{i}") for i in range(2)]
                for p in range(3):
                    tk.dma(st_kv[0], kT[:, p, :], skT_d[:, p, :], R=r_sk, W=[r_kT])
                vsrc = sv_d.rearrange("(b p) c -> p b c", p=128)
                bstep = 8
                for b0 in range(0, NT, bstep):
                    b1 = min(NT, b0 + bstep)
                    tk.dma(st_kv[1], vA[:, b0:b1, :], vsrc[:, b0:b1, :], R=r_sv, W=[r_vA])
                with ExitStack() as ws:
                    wst = [ws.enter_context(nc.sbuf_tensor(f"c{l}_wst{i}", [128, 8, 256], F32)) for i in range(2)]
                    r_wst = [Res("wst0"), Res("wst1")]
                    srcv = wout_d[l].rearrange("(k p) c -> p k c", p=128)
                    for ci in range(4):
                        c0 = ci * 256
                        s = ci % 2
                        tk.dma(st_misc[1 + s], wst[s][:], srcv[:, :, c0:c0 + 256], W=[r_wst[s]])
                        if ci % 2:
                            tk.act(lambda e, s=s, c0=c0: e.copy(out=wout[:, :, c0:c0 + 256], in_=wst[s][:]),
                                   R=[r_wst[s]], W=[r_wo])
                        else:
                            tk.dve(lambda e, s=s, c0=c0: e.tensor_copy(out=wout[:, :, c0:c0 + 256], in_=wst[s][:]),
                                   R=[r_wst[s]], W=[r_wo])
                    tk.barrier()
                r_wo = Res("wout2")
                qTg = [sbt(f"qTg{i}", [128, 3, 512], BF16) for i in range(2)]
                r_qTg = [Res("qTg0"), Res("qTg1")]
                st_q = [tk.stream(f"c_q{i}") for i in range(2)]
                NMP = 2
                mp = [sbt(f"mp{i}", [128, 4, 512], BF16) for i in range(NMP)]
                r_mp = [Res(f"mp{i}") for i in range(NMP)]
                st_mp = [tk.stream(f"c_mp{i}") for i in range(NMP)]
                NE = 6
                esb = [sbt(f"esb{i}", [128, 512], BF16) for i in range(NE)]
                r_esb = [Res(f"esb{i}") for i in range(NE)]
                pmb = [sbt(f"pmb{i}", [128, 512], BF16) for i in range(NE)]
                r_pmb = [Res(f"pmb{i}") for i in range(NE)]
                osb = sbt("osb", [65, 6, 512], F32)
                r_osb = Res("osb")
                rinv = sbt("rinv", [128, 8], F32)
                r_rinv = Res("rinv")
                ymx = [sbt(f"ymx{i}", [128, D], BF16) for i in range(2)]
                r_ymx = [Res("ymx0"), Res("ymx1")]
                st_ym = [tk.stream(f"c_ym{i}") for i in range(2)]
                yT = sbt("yT", [128, 8, 128], BF16)
                r_yT = Res("yT")
                xt = [sbt(f"xt{i}", [128, D], F32) for i in range(2)]
                r_xt = [Res("xt0"), Res("xt1")]
                st_xl = [tk.stream(f"c_xl{i}") for i in range(2)]
                st_xs = [tk.stream(f"c_xs{i}") for i in range(2)]
                fst = sbt("fst", [128, 8], F32)
                r_fst = Res("fst")
                QK = (6, 7)
                qkbf = banks[7].bitcast(BF16)
                cnt = [0, 0]
                pend = []
                for g in range(NG):
                    gs = g % 2
                    nkb = 4 * g + 4
                    tk.dma(st_q[gs], qTg[gs][:], sqT_d[:, :, g * 512:(g + 1) * 512], R=[r_sq[g]], W=[r_qTg[gs]])
                    for kc in range(g + 1):
                        ms = cnt[1] % NMP
                        cnt[1] += 1
                        tk.dma(st_mp[ms], mp[ms][:], smk_d[g, kc * 4:(kc + 1) * 4, :, :].rearrange("k p q -> p k q"),
                               R=r_smk[4 * g:4 * g + 4], W=[r_mp[ms]])
                        for kbi in range(4):
                            kb = kc * 4 + kbi
                            for p in range(3):
                                slots = []
                                for hh in range(2):
                                    h, base = 2 * p + hh, 64 * hh
                                    i = cnt[0]
                                    cnt[0] += 1
                                    qb_ = QK[hh]
                                    es_ = i % NE
                                    slots.append((h, qb_, es_))
                                    tk.pe(lambda e, base=base, qb_=qb_: e.matmul(
                                        out=banks[qb_][:, :], lhsT=kT[base:base + 64, p, kb * 128:(kb + 1) * 128],
                                        rhs=qTg[gs][base:base + 64, p, :], start=True, stop=True),
                                        R=[r_kT, r_qTg[gs]], W=[rbank[qb_]])
                                for (h, qb_, es_) in slots:
                                    for j in range(4):
                                        d = 4 * g + j - kb
                                        if d == 0 or d == 1:
                                            off = G_BD if d == 0 else G_BP
                                            tk.dve(lambda e, j=j, off=off, h=h, qb_=qb_: e.tensor_tensor(
                                                out=banks[qb_][:, j * 128:(j + 1) * 128],
                                                in0=banks[qb_][:, j * 128:(j + 1) * 128],
                                                in1=gpk[:, off + h * 128:off + (h + 1) * 128], op=ALU.add),
                                                R=[r_gpk], W=[rbank[qb_]])
                                    tk.act(lambda e, h=h, qb_=qb_, es_=es_: e.activation(
                                        out=esb[es_][:], in_=banks[qb_][:, :], func=AF.Exp,
                                        bias=gpk[:, G_C31 + h:G_C31 + h + 1], scale=0.125),
                                        R=[rbank[qb_], r_gpk], W=[r_esb[es_]])
                                    tk.dve(lambda e, es_=es_: e.tensor_tensor(out=pmb[es_][:], in0=esb[es_][:],
                                                                              in1=mp[ms][:, kbi, :], op=ALU.mult),
                                           R=[r_esb[es_], r_mp[ms]], W=[r_pmb[es_]])

                                    def pv(h=h, kb=kb, es_=es_):
                                        tk.pe(lambda e: e.matmul(
                                            out=banks[h][0:65, :], lhsT=vA[:, kb, h * 65:(h + 1) * 65], rhs=pmb[es_][:],
                                            start=(kb == 0), stop=(kb == nkb - 1)),
                                            R=[r_vA, r_pmb[es_]], W=[rbank[h]])
                                    pend.append(pv)
                                while len(pend) > 4:
                                    pend.pop(0)()
                    while pend:
                        pend.pop(0)()
                    for h in range(6):
                        if h % 2:
                            tk.act(lambda e, h=h: e.copy(out=osb[0:65, h, :], in_=banks[h][0:65, :]),
                                   R=[rbank[h]], W=[r_osb])
                        else:
                            tk.dve(lambda e, h=h: e.tensor_copy(out=osb[0:65, h, :], in_=banks[h][0:65, :]),
                                   R=[rbank[h]], W=[r_osb])
                    for j in range(4):
                        qb = 4 * g + j
                        s = qb % 2
                        jsl = slice(j * 128, (j + 1) * 128)
                        rsl = slice(qb * 128, (qb + 1) * 128)
                        tk.dma(st_ym[s], ymx[s][:], sym_d[rsl, :], R=[r_sym[qb]], W=[r_ymx[s]])
                        tk.dma(st_xl[s], xt[s][:], x_src[rsl, :], R=[r_x[qb]], W=[r_xt[s]])
                        for h in range(6):
                            tk.pe(lambda e, h=h: e.transpose(out=banks[6][:, h * 65:(h + 1) * 65], in_=osb[0:65, h, jsl],
                                                             identity=cst[0:65, C_ID:C_ID + 65]),
                                  R=[r_osb, r_cst], W=[rbank[6]])
                        tk.dve(lambda e: e.reciprocal(
                            out=rinv[:, 0:6],
                            in_=banks[6][:, 0:390].rearrange("p (h d) -> p h d", d=65)[:, :, 64]),
                            R=[rbank[6]], W=[r_rinv])
                        for h in range(6):
                            ysl = slice(384 + h * 64, 384 + (h + 1) * 64)
                            tk.dve(lambda e, h=h, ysl=ysl: e.scalar_tensor_tensor(
                                out=ymx[s][:, ysl], in0=banks[6][:, h * 65:h * 65 + 64], scalar=rinv[:, h:h + 1],
                                in1=ymx[s][:, ysl], op0=ALU.mult, op1=ALU.mult),
                                R=[rbank[6], r_rinv, r_ymx[s]], W=[r_ymx[s]])
                        for k in range(8):
                            tk.pe(lambda e, k=k: e.transpose(out=qkbf[:, k * 128:(k + 1) * 128],
                                                             in_=ymx[s][:, k * 128:(k + 1) * 128], identity=IDb),
                                  R=[r_ymx[s], r_cbf], W=[rbank[7]])
                        tk.act(lambda e: e.copy(out=yT[:], in_=qkbf[:, :].rearrange("p (k c) -> p k c", k=8)),
                               R=[rbank[7]], W=[r_yT])
                        for n in range(2):
                            for k in range(8):
                                tk.pe(lambda e, n=n, k=k: e.matmul(
                                    out=banks[n][:, :], lhsT=yT[:, k, :], rhs=wout[:, k, n * 512:(n + 1) * 512],
                                    start=(k == 0), stop=(k == 7)), R=[r_yT, r_wo], W=[rbank[n]])
                            tk.dve(lambda e, n=n: e.tensor_tensor(
                                out=xt[s][:, n * 512:(n + 1) * 512], in0=xt[s][:, n * 512:(n + 1) * 512],
                                in1=banks[n][:, :], op=ALU.add), R=[rbank[n], r_xt[s]], W=[r_xt[s]])
                        if not last:
                            tk.dma(st_xs[s], xres_d[rsl, :], xt[s][:], R=[r_xt[s]], W=[r_x[qb]])
                        else:
                            tk.act(lambda e: e.activation(out=yT[:].rearrange("p k c -> p (k c)"), in_=xt[s][:],
                                                          func=AF.Square, accum_out=fst[:, 0:1]),
                                   R=[r_xt[s]], W=[r_fst, r_yT])
                            tk.act(lambda e: e.activation(out=fst[:, 1:2], in_=fst[:, 0:1], func=AF.Sqrt,
                                                          bias=epst[:, 0:1], scale=1.0 / D),
                                   R=[r_fst, r_eps], W=[r_fst])
                            tk.dve(lambda e: e.reciprocal(out=fst[:, 2:3], in_=fst[:, 1:2]), R=[r_fst], W=[r_fst])
                            tk.dve(lambda e: e.scalar_tensor_tensor(
                                out=xt[s][:], in0=xt[s][:], scalar=fst[:, 2:3], in1=gpk[:, G_FN:G_FN + D],
                                op0=ALU.mult, op1=ALU.mult), R=[r_xt[s], r_fst, r_gpk], W=[r_xt[s]])
                            tk.dma(st_xs[s], y_d[rsl, :], xt[s][:], R=[r_xt[s]], W=[r_x[qb]])
                tk.barrier()

            if stop_after == "A":
                stopped[0] = True
                return
            with ExitStack() as pb:
                phase_b1(pb)
            if stop_after == "B1":
                stopped[0] = True
                return
            with ExitStack() as pb:
                phase_b2(pb)

        stopped = [False]
        for l in range(depth if stop_after != "init" else 0):
            layer(l)
            if stopped[0]:
                break

        tk.barrier()
    print("instructions emitted:", tk.ninst)
    return nc


def prep_inputs(x, norm_g, w_in, gla_gate_w2, gla_gate_b, gla_norm_g, rel_bias, pool_w, pool_scale,
                w_out, final_norm_g, depth=DEPTH):
    f = np.float32
    w_in = np.asarray(w_in, f)
    wfm = np.zeros((depth, D, NFM), f)
    wtm = np.zeros((depth, D, NTM), f)
    lp = np.zeros((depth, 128, NPL), f)
    for l in range(depth):
        wfm[l], wtm[l] = _w_layouts(w_in[l])
        lp[l] = _layer_pack(np.asarray(norm_g[l], f), np.asarray(gla_norm_g[l], f),
                            np.asarray(pool_scale[l], f), np.asarray(gla_gate_w2[l], f),
                            np.asarray(gla_gate_b[l], f), np.asarray(pool_w[l], f))
    gp = _global_pack(np.asarray(rel_bias, f), np.asarray(final_norm_g, f))
    shared = {"wfm": wfm, "wtm": wtm, "wout": np.ascontiguousarray(np.asarray(w_out, f)[:depth]),
              "lpack": lp, "gpack": gp, "cpack": _consts()}
    return shared


def kernel(x, norm_g, w_in, gla_gate_w2, gla_gate_b, gla_norm_g, rel_bias, pool_w, pool_scale,
           w_out, final_norm_g):
    x = np.asarray(x, np.float32)
    B, T, _ = x.shape
    shared = prep_inputs(x, norm_g, w_in, gla_gate_w2, gla_gate_b, gla_norm_g, rel_bias, pool_w,
                         pool_scale, w_out, final_norm_g)
    nc = build(T=T, depth=DEPTH)
    in_maps = [dict(shared, x=np.ascontiguousarray(x[b])) for b in range(B)]
    res = run_bass_kernel_spmd(nc, in_maps, core_ids=list(range(B)))
    return np.stack([np.asarray(r["y"], np.float32) for r in res.results], axis=0)
```
